# Optimizing a Trainium2 kernel written in Bass

```python
import jax, jax.numpy as jnp
from jax import lax
import numpy as np

D_MODEL = 1024
BATCH = 4
SEQ = 8192
DEPTH = 1

SSD_EXPAND = 2
SSD_D_INNER = SSD_EXPAND * D_MODEL
SSD_HEAD_DIM = 64
SSD_N_HEADS = SSD_D_INNER // SSD_HEAD_DIM
SSD_N_GROUPS = 4
SSD_D_STATE = 128
SSD_CONV = 4
SSD_CHUNK = 128
SSD_CONV_DIM = SSD_D_INNER + 2 * SSD_N_GROUPS * SSD_D_STATE

ATTN_HEAD_DIM = 64
ATTN_N_HEADS = 16
ATTN_WIDTH = ATTN_N_HEADS * ATTN_HEAD_DIM
MOBA_BLOCK = 256
MOBA_TOPK = 3
MOBA_Q_CHUNK = 32

FFN_HIDDEN = 4 * D_MODEL
FFN_CONV = 3

NORM_EPS = 1e-6

IN_SIZES = (SSD_D_INNER, SSD_CONV_DIM, SSD_N_HEADS, ATTN_WIDTH, ATTN_WIDTH, ATTN_WIDTH, D_MODEL, D_MODEL)
IN_COLS = sum(IN_SIZES)
IN_OFFSETS = tuple(sum(IN_SIZES[:j]) for j in range(1, len(IN_SIZES)))

kernel_name = 'hybrid_ssd_moba_block'


def rms_norm(x, w):
    xf = x.astype(jnp.float32)
    y = xf * lax.rsqrt(jnp.mean(xf * xf, axis=-1, keepdims=True) + NORM_EPS)
    return (y * w.astype(jnp.float32)).astype(x.dtype)


def group_rms_norm(x, w, groups):
    shp = x.shape
    xf = x.astype(jnp.float32).reshape(shp[:-1] + (groups, shp[-1] // groups))
    y = xf * lax.rsqrt(jnp.mean(xf * xf, axis=-1, keepdims=True) + NORM_EPS)
    return (y.reshape(shp) * w.astype(jnp.float32)).astype(x.dtype)


def causal_dwconv(x, w, b):
    width, ch = w.shape
    y = lax.conv_general_dilated(x, w[:, None, :].astype(x.dtype), window_strides=(1,),
                                 padding=[(width - 1, 0)],
                                 dimension_numbers=('NWC', 'WIO', 'NWC'),
                                 feature_group_count=ch)
    return y + b.astype(x.dtype)


def ssd_chunked_scan(x, dt, a, bm, cm):
    b, s, h, p = x.shape
    g, n = bm.shape[-2:]
    r = h // g
    nc = s // SSD_CHUNK
    xdt = (x * dt[..., None]).reshape(b, nc, SSD_CHUNK, g, r, p)
    adt = (dt * a).reshape(b, nc, SSD_CHUNK, g, r)
    bc = bm.reshape(b, nc, SSD_CHUNK, g, n)
    cc = cm.reshape(b, nc, SSD_CHUNK, g, n)
    xs = (jnp.moveaxis(xdt, 1, 0), jnp.moveaxis(adt, 1, 0), jnp.moveaxis(bc, 1, 0), jnp.moveaxis(cc, 1, 0))
    causal = jnp.tril(jnp.ones((SSD_CHUNK, SSD_CHUNK), dtype=bool))

    def step(state, inp):
        xc, ac, bch, cch = inp
        acs = jnp.cumsum(ac, axis=1)
        seg = acs[:, :, None] - acs[:, None, :]
        decay = jnp.exp(jnp.where(causal[None, :, :, None, None], seg, -jnp.inf))
        cb = jnp.einsum('blgn,bsgn->blsg', cch, bch)
        y = jnp.einsum('blsg,blsgr,bsgrp->blgrp', cb, decay, xc)
        y = y + jnp.einsum('blgn,bgrpn->blgrp', cch, state) * jnp.exp(acs)[..., None]
        last = acs[:, -1]
        w_end = jnp.exp(last[:, None] - acs)
        state = state * jnp.exp(last)[..., None, None] + jnp.einsum('blgn,blgr,blgrp->bgrpn', bch, w_end, xc)
        return state, y

    state0 = jnp.zeros((b, g, r, p, n), jnp.float32)
    _, ys = lax.scan(step, state0, xs)
    return jnp.moveaxis(ys, 0, 1).reshape(b, s, h, p)


def alibi_slopes(n_heads):
    return 2.0 ** (-8.0 * jnp.arange(1, n_heads + 1, dtype=jnp.float32) / n_heads)


def moba_attention(q, k, v):
    b, s, h, dh = q.shape
    sp = -(-s // MOBA_BLOCK) * MOBA_BLOCK
    nb = sp // MOBA_BLOCK
    k_sel = min(MOBA_TOPK, nb)
    pad = [(0, 0), (0, sp - s), (0, 0), (0, 0)]
    qp = jnp.pad(q, pad).transpose(0, 2, 1, 3)
    kb = jnp.pad(k, pad).transpose(0, 2, 1, 3).reshape(b, h, nb, MOBA_BLOCK, dh)
    vb = jnp.pad(v, pad).transpose(0, 2, 1, 3).reshape(b, h, nb, MOBA_BLOCK, dh)
    kmean = jnp.mean(kb.astype(jnp.float32), axis=3)
    slopes = alibi_slopes(h)
    scale = dh ** -0.5
    offs = jnp.arange(MOBA_BLOCK)
    gather_blocks = jax.vmap(jax.vmap(lambda blocks, idx: blocks[idx]))

    def chunk(c):
        q0 = c * MOBA_Q_CHUNK
        qc = lax.dynamic_slice_in_dim(qp, q0, MOBA_Q_CHUNK, axis=2).astype(jnp.float32)
        qpos = q0 + jnp.arange(MOBA_Q_CHUNK)
        own = q0 // MOBA_BLOCK
        gate = jnp.einsum('bhqd,bhnd->bhqn', qc, kmean)
        gate = jnp.where(jnp.arange(nb) < own, gate, -jnp.inf)
        top_s, top_i = lax.top_k(gate, k_sel)
        ksel = gather_blocks(kb, top_i).astype(jnp.float32)
        vsel = gather_blocks(vb, top_i).astype(jnp.float32)
        kpos = top_i[..., None] * MOBA_BLOCK + offs
        s_sel = (jnp.einsum('bhqd,bhqjsd->bhqjs', qc, ksel) * scale
                 - slopes[None, :, None, None, None] * (qpos[:, None, None] - kpos).astype(jnp.float32))
        s_sel = jnp.where(jnp.isfinite(top_s)[..., None], s_sel, -jnp.inf)
        ko = lax.dynamic_index_in_dim(kb, own, axis=2, keepdims=False).astype(jnp.float32)
        vo = lax.dynamic_index_in_dim(vb, own, axis=2, keepdims=False).astype(jnp.float32)
        kpos_o = own * MOBA_BLOCK + offs
        s_own = (jnp.einsum('bhqd,bhsd->bhqs', qc, ko) * scale
                 - slopes[None, :, None, None] * (qpos[:, None] - kpos_o[None, :]).astype(jnp.float32))
        s_own = jnp.where(kpos_o[None, :] <= qpos[:, None], s_own, -jnp.inf)
        n_sel = k_sel * MOBA_BLOCK
        scores = jnp.concatenate([s_sel.reshape(b, h, MOBA_Q_CHUNK, n_sel), s_own], axis=-1)
        prob = jax.nn.softmax(scores, axis=-1)
        p_sel = prob[..., :n_sel].reshape(b, h, MOBA_Q_CHUNK, k_sel, MOBA_BLOCK)
        p_own = prob[..., n_sel:]
        out = jnp.einsum('bhqjs,bhqjsd->bhqd', p_sel, vsel) + jnp.einsum('bhqs,bhsd->bhqd', p_own, vo)
        return out.astype(q.dtype)

    outs = lax.map(chunk, jnp.arange(sp // MOBA_Q_CHUNK))
    out = outs.transpose(1, 0, 3, 2, 4).reshape(b, sp, h, dh)
    return out[:, :s]


def setup_inputs(seed: int = 0) -> dict:
    key = jax.random.key(seed)
    ks = jax.random.split(key, 24)
    f32 = jnp.float32

    def dense(k, fan_in, fan_out):
        return jax.random.normal(k, (DEPTH, fan_in, fan_out), f32) * fan_in ** -0.5

    def gain(k, n):
        return 1.0 + 0.05 * jax.random.normal(k, (DEPTH, n), f32)

    dt0 = jnp.exp(jax.random.uniform(ks[5], (DEPTH, SSD_N_HEADS), f32) * (np.log(0.1) - np.log(0.001)) + np.log(0.001))
    dt_bias = dt0 + jnp.log(-jnp.expm1(-dt0))
    return {
        'x': jax.random.normal(ks[0], (BATCH, SEQ, D_MODEL), f32),
        'pre_mix_norm': gain(ks[1], D_MODEL),
        'w_in': dense(ks[2], D_MODEL, IN_COLS),
        'ssd_conv_w': jax.random.normal(ks[3], (DEPTH, SSD_CONV, SSD_CONV_DIM), f32) * SSD_CONV ** -0.5,
        'ssd_conv_b': 0.01 * jax.random.normal(ks[4], (DEPTH, SSD_CONV_DIM), f32),
        'ssd_dt_bias': dt_bias,
        'ssd_a_log': jnp.log(jax.random.uniform(ks[6], (DEPTH, SSD_N_HEADS), f32, 1.0, 16.0)),
        'ssd_d_skip': 1.0 + 0.1 * jax.random.normal(ks[7], (DEPTH, SSD_N_HEADS), f32),
        'ssd_out_norm': gain(ks[8], SSD_D_INNER),
        'w_ssd_branch': dense(ks[9], SSD_D_INNER, D_MODEL),
        'w_attn_branch': dense(ks[10], ATTN_WIDTH, D_MODEL),
        'w_out': dense(ks[11], D_MODEL, D_MODEL),
        'post_mix_norm': gain(ks[12], D_MODEL),
        'pre_ffn_norm': gain(ks[13], D_MODEL),
        'w_ffn_up': dense(ks[14], D_MODEL, 2 * FFN_HIDDEN),
        'ffn_conv_w': jax.random.normal(ks[15], (DEPTH, FFN_CONV, FFN_HIDDEN), f32) * FFN_CONV ** -0.5,
        'ffn_conv_b': 0.01 * jax.random.normal(ks[16], (DEPTH, FFN_HIDDEN), f32),
        'w_ffn_down': dense(ks[17], FFN_HIDDEN, D_MODEL),
        'post_ffn_norm': gain(ks[18], D_MODEL),
    }


def reference(x, pre_mix_norm, w_in, ssd_conv_w, ssd_conv_b, ssd_dt_bias, ssd_a_log, ssd_d_skip,
              ssd_out_norm, w_ssd_branch, w_attn_branch, w_out, post_mix_norm, pre_ffn_norm,
              w_ffn_up, ffn_conv_w, ffn_conv_b, w_ffn_down, post_ffn_norm):
    b, s, _ = x.shape
    gn = SSD_N_GROUPS * SSD_D_STATE
    for i in range(DEPTH):
        h = rms_norm(x, pre_mix_norm[i])
        proj = h @ w_in[i]
        z, xbc, dt_raw, q, k, v, g_ssd, g_attn = jnp.split(proj, IN_OFFSETS, axis=-1)

        xbc = jax.nn.silu(causal_dwconv(xbc, ssd_conv_w[i], ssd_conv_b[i]))
        xs, bm, cm = jnp.split(xbc, [SSD_D_INNER, SSD_D_INNER + gn], axis=-1)
        xs4 = xs.reshape(b, s, SSD_N_HEADS, SSD_HEAD_DIM).astype(jnp.float32)
        dt = jax.nn.softplus((dt_raw + ssd_dt_bias[i]).astype(jnp.float32))
        a = -jnp.exp(ssd_a_log[i].astype(jnp.float32))
        y = ssd_chunked_scan(xs4, dt, a,
                             bm.reshape(b, s, SSD_N_GROUPS, SSD_D_STATE).astype(jnp.float32),
                             cm.reshape(b, s, SSD_N_GROUPS, SSD_D_STATE).astype(jnp.float32))
        y = y + ssd_d_skip[i].astype(jnp.float32)[:, None] * xs4
        y = y.reshape(b, s, SSD_D_INNER).astype(x.dtype)
        y = group_rms_norm(y * jax.nn.silu(z), ssd_out_norm[i], SSD_N_GROUPS)
        y_ssd = y @ w_ssd_branch[i]

        att = moba_attention(q.reshape(b, s, ATTN_N_HEADS, ATTN_HEAD_DIM),
                             k.reshape(b, s, ATTN_N_HEADS, ATTN_HEAD_DIM),
                             v.reshape(b, s, ATTN_N_HEADS, ATTN_HEAD_DIM))
        y_attn = att.reshape(b, s, ATTN_WIDTH) @ w_attn_branch[i]

        mixed = jax.nn.sigmoid(g_ssd) * y_ssd + jax.nn.sigmoid(g_attn) * y_attn
        x = x + rms_norm(mixed @ w_out[i], post_mix_norm[i])

        h = rms_norm(x, pre_ffn_norm[i])
        gate, up = jnp.split(h @ w_ffn_up[i], 2, axis=-1)
        gate = causal_dwconv(gate, ffn_conv_w[i], ffn_conv_b[i])
        ff = (jax.nn.gelu(gate, approximate=True) * up) @ w_ffn_down[i]
        x = x + rms_norm(ff, post_ffn_norm[i])
    return x
```

```python
import numpy as np
import ml_dtypes
from contextlib import ExitStack
import concourse.bass as bass
import concourse.mybir as mybir
from concourse.bass_utils import run_bass_kernel_spmd

F32 = mybir.dt.float32
BF16 = mybir.dt.bfloat16
AF = mybir.ActivationFunctionType
ALU = mybir.AluOpType
AX = mybir.AxisListType
PE, ACT, DVE, POOL, SP = "tensor", "scalar", "vector", "gpsimd", "sync"
ENGS = (PE, ACT, DVE, POOL, SP)
NPBF = ml_dtypes.bfloat16

D = 1024
TT = 512
NCOL_IN = 10272
OFF_Z, OFF_XBC, OFF_DT, OFF_Q, OFF_K, OFF_V, OFF_GS, OFF_GA = 0, 2048, 5120, 5152, 6176, 7200, 8224, 9248
BIG = 30000.0
EPS = 1e-6
KU = 1024
NSU = KU // 128
SLOPES = [2.0 ** (-8.0 * (h + 1) / 16) for h in range(16)]
ALIBI_CUT = 150.0
SEMI_SHORT = True


class Buf:
    __slots__ = ("ap", "w", "r", "name", "root", "psum")

    def __init__(self, ap, name="", root=None, psum=False):
        self.ap = ap
        self.w = None
        self.r = []
        self.name = name
        self.root = root.root if root is not None else self
        self.psum = psum or (root is not None and root.root.psum)


class Op:
    __slots__ = ("eng", "fn", "deps", "sig", "ms", "dma", "sem", "target")

    def __init__(self, eng, fn, deps, dma):
        self.eng, self.fn, self.deps, self.dma = eng, fn, deps, dma
        self.sig, self.ms, self.sem, self.target = False, 0, None, 0


class Prog:
    def __init__(self, nc, n_dma_sems=14):
        self.nc = nc
        self.ops = {e: [] for e in ENGS}
        self.n_dma_sems = n_dma_sems
        self.dma_rr = {e: 0 for e in ENGS}
        self.dma_cnt = {}
        self.dma_last = {}

    def emit(self, eng, fn, reads=(), writes=(), dma=False, same_ok=False):
        deps, seen = [], set()
        reads_, writes_ = [], []
        for b in reads:
            (writes_ if b.root.psum else reads_).append(b.root)
        for b in writes:
            writes_.append(b.root)
        reads, writes = reads_, writes_
        touched = [b for b in reads + writes if not b.psum]
        cand = []
        for b in reads:
            if b.w is not None:
                cand.append(b.w)
        for b in writes:
            if b.w is not None:
                cand.append(b.w)
            cand.extend(b.r)
        for d in cand:
            if id(d) in seen:
                continue
            seen.add(id(d))
            if d.eng == eng and not d.dma:
                if eng == PE or eng == SP or same_ok:
                    continue
            deps.append(d)
        op = Op(eng, fn, deps, dma)
        if dma:
            slot = self.dma_rr[eng] % self.n_dma_sems
            self.dma_rr[eng] += 1
            key = (eng, slot)
            prev = self.dma_last.get(key)
            if prev is not None:
                op.deps.append(prev)
            self.dma_cnt[key] = self.dma_cnt.get(key, 0) + 1
            op.sem, op.target = key, 16 * self.dma_cnt[key]
            self.dma_last[key] = op
        for d in op.deps:
            d.sig = True
        for b in reads:
            b.r.append(op)
        for b in writes:
            b.w = op
            b.r = []
        self.ops[eng].append(op)
        return op

    def finalize(self, final_ops):
        nc = self.nc
        for o in final_ops:
            o.sig = True
        for e in ENGS:
            c = 0
            for op in self.ops[e]:
                if not op.dma and op.sig:
                    c += 1
                    op.ms = c
        with ExitStack() as st:
            esem = {e: st.enter_context(nc.semaphore("s_" + e)) for e in ENGS}
            dsem = {k: st.enter_context(nc.semaphore("d_%s_%d" % k)) for k in self.dma_cnt}
            block = st.enter_context(nc.Block())

            def run(e, h, last=False):
                waited = {}
                for op in self.ops[e]:
                    for d in op.deps:
                        if d.dma:
                            s, v, k = dsem[d.sem], d.target, ("d",) + d.sem
                        else:
                            s, v, k = esem[d.eng], d.ms, ("e", d.eng)
                        if waited.get(k, 0) >= v:
                            continue
                        waited[k] = v
                        h.wait_ge(s, v)
                    ins = op.fn(h)
                    if op.dma:
                        ins.then_inc(dsem[op.sem], 16)
                    elif op.sig:
                        ins.then_inc(esem[e], 1)
                if last:
                    for d in final_ops:
                        if d.dma:
                            h.wait_ge(dsem[d.sem], d.target)
                        else:
                            h.wait_ge(esem[d.eng], d.ms)

            block.tensor(lambda h: run(PE, h))
            block.scalar(lambda h: run(ACT, h))
            block.vector(lambda h: run(DVE, h))
            block.gpsimd(lambda h: run(POOL, h))
            block.sync(lambda h: run(SP, h, last=True))
        return {e: len(self.ops[e]) for e in ENGS}


def build_nc(NP, NM, dbg=(), dbg_tile=None):
    W = (NP + NM) * TT
    NPB, NLB = 2 * NP, 2 * NM
    NOFFMAX = (NP + NM - 1) * 4
    NOFF = NOFFMAX + 4
    NUNITS = (W + KU - 1) // KU
    nc = bass.Bass("TRN2", target_bir_lowering=False)
    P = Prog(nc)
    din = lambda n, s, d: nc.dram_tensor(n, list(s), d, kind="ExternalInput").ap()
    dsc = lambda n, s, d: nc.dram_tensor(n, list(s), d).ap()

    xw = din("xw", [W, D], F32)
    w_in = din("w_in", [D, NCOL_IN], F32)
    w_ssd = din("w_ssd", [2048, D], F32)
    w_attn = din("w_attn", [D, D], F32)
    w_out = din("w_out", [D, D], F32)
    w_up = din("w_up", [D, 8192], F32)
    w_down = din("w_down", [4096, D], F32)
    g_pre_mix = din("g_pre_mix", [128, 8], F32)
    g_pre_ffn = din("g_pre_ffn", [128, 8], F32)
    g_post_mix = din("g_post_mix", [1, D], F32)
    g_post_ffn = din("g_post_ffn", [1, D], F32)
    cw_d = din("cw", [128, 24, 4], F32)
    cb_d = din("cb", [128, 24], F32)
    dtb_d = din("dtb", [64, 1], F32)
    alog_d = din("alog", [64, 1], F32)
    dsk_d = din("dsk", [128, 16], F32)
    gon_d = din("gon", [128, 16], F32)
    fcw_d = din("fcw", [128, 32, 3], F32)
    fcb_d = din("fcb", [128, 32], F32)
    identb_d = din("identb", [128, 128], BF16)
    identf_d = din("identf", [64, 64], F32)
    selcol_d = din("selcol", [64, 32], F32)
    triu_d = din("triu", [128, 128], F32)
    cmask_d = din("cmask", [128, 4, 512], BF16)
    kaug_d = din("kaug", [33, NUNITS * KU], BF16)
    qsh_d = din("qsh", [128, 16, 4], F32)
    abias_d = din("abias", [128, 16, NOFF], F32)
    vtab_d = din("vtab", [128, NLB + 2, 32], F32)
    own_d = din("own01", [128, NLB + 2, 32], F32)
    pv_d = din("pv", [128, 1], F32)
    out_d = nc.dram_tensor("out", [NM * TT, D], F32, kind="ExternalOutput").ap()
    dbg_out = {}

    wb_in = dsc("wb_in", [D, NCOL_IN], BF16)
    wb_ssd = dsc("wb_ssd", [2048, D], BF16)
    wb_attn = dsc("wb_attn", [D, D], BF16)
    wb_out = dsc("wb_out", [D, D], BF16)
    wb_up = dsc("wb_up", [D, 8192], BF16)
    wb_down = dsc("wb_down", [4096, D], BF16)
    Ks = dsc("Ks", [16, 64, NUNITS * KU], BF16)
    Vs = dsc("Vs", [16, 128, NUNITS * NSU, 64], BF16)

    st = ExitStack()
    with st:
        def sb(name, shape, dt):
            return Buf(st.enter_context(nc.sbuf_tensor("sb_" + name, list(shape), dt)), name)

        def ps(name, shape, dt=F32):
            return Buf(st.enter_context(nc.psum_tensor("ps_" + name, list(shape), dt)), name, psum=True)

        def sub(b, ap, name=""):
            return Buf(ap, name)

        DIN = Buf(None, "dram_in")
        wbB = {}

        def E(eng, fn, reads=(), writes=(), **kw):
            return P.emit(eng, fn, reads=reads, writes=writes, **kw)

        def dma(eng, out_ap, in_ap, reads, writes):
            return P.emit(eng, lambda e: e.dma_start(out=out_ap, in_=in_ap), reads=reads, writes=writes, dma=True)

        identb = sb("identb", [128, 128], BF16)
        identf = sb("identf", [64, 64], F32)
        selcol = sb("selcol", [64, 32], F32)
        triu = sb("triu", [128, 128], F32)
        onesb = sb("onesb", [128, 128], BF16)
        onesf = sb("onesf", [128, 128], F32)
        cmask = sb("cmask", [128, 4, 512], BF16)
        qsh = sb("qsh", [128, 16, 4], F32)
        abias = sb("abias", [128, 16, NOFF], F32)
        vtab = sb("vtab", [128, NLB + 2, 32], F32)
        own01 = sb("own01", [128, NLB + 2, 32], F32)
        pv = sb("pv", [128, 1], F32)
        gpm = sb("gpm", [128, 8], F32)
        gpf = sb("gpf", [128, 8], F32)
        gqm = sb("gqm", [128, D], F32)
        gqf = sb("gqf", [128, D], F32)
        cw = sb("cw", [128, 24, 4], F32)
        cb = sb("cb", [128, 24], F32)
        dtb = sb("dtb", [64, 1], F32)
        aneg = sb("aneg", [64, 1], F32)
        dsk = sb("dsk", [128, 16], F32)
        gon = sb("gon", [128, 16], F32)
        fcw = sb("fcw", [128, 32, 3], F32)
        fcb = sb("fcb", [128, 32], F32)
        for t, s in ((identb, identb_d), (identf, identf_d), (selcol, selcol_d), (triu, triu_d), (cmask, cmask_d), (qsh, qsh_d),
                     (abias, abias_d), (vtab, vtab_d), (own01, own_d), (pv, pv_d), (gpm, g_pre_mix), (gpf, g_pre_ffn),
                     (cw, cw_d), (cb, cb_d), (dtb, dtb_d), (aneg, alog_d), (dsk, dsk_d), (gon, gon_d), (fcw, fcw_d), (fcb, fcb_d)):
            dma(SP, t.ap[:], s, [DIN], [t])
        dma(SP, gqm.ap[:], g_post_mix.partition_broadcast(128), [DIN], [gqm])
        dma(SP, gqf.ap[:], g_post_ffn.partition_broadcast(128), [DIN], [gqf])
        E(DVE, lambda e: e.memset(onesb.ap[:], 1.0), writes=[onesb])
        E(DVE, lambda e: e.memset(onesf.ap[:], 1.0), writes=[onesf])
        E(ACT, lambda e: e.activation(out=aneg.ap[:], in_=aneg.ap[:], func=AF.Exp), reads=[aneg], writes=[aneg])
        E(DVE, lambda e: e.tensor_scalar(out=aneg.ap[:], in0=aneg.ap[:], scalar1=-1.0, scalar2=None, op0=ALU.mult), reads=[aneg], writes=[aneg])

        xt = sb("xt", [128, 4, D], F32)
        xt_s = [Buf(xt.ap[:, s_, :]) for s_ in range(4)]
        hT = sb("hT", [128, 8, TT], BF16)
        junk = sb("junk", [128, D], BF16)
        ssq = sb("ssq", [128, 4], F32)
        rstd = sb("rstd", [128, 4], F32)
        Gall = sb("Gall", [128, 32, TT], BF16)
        G = [Buf(Gall.ap[:, i, :], "G%d" % i) for i in range(32)]
        xbc, qT, act = G[0:24], G[24:32], G
        Hall = sb("Hall", [128, 16, TT], BF16)
        yT = [Buf(Hall.ap[:, i, :], "yT%d" % i) for i in range(16)]
        attall = sb("attall", [64, 16, TT], BF16)
        attT = [Buf(attall.ap[:, i, :], "att%d" % i) for i in range(16)]
        NWB = 3
        wbufs = [sb("wbuf%d" % i, [128, 4096], BF16) for i in range(NWB)]
        fT = [sb("fT%d" % i, [128, TT], F32) for i in range(4)]
        stage = [sb("stage%d" % i, [128, TT + 3], BF16) for i in range(3)]
        stage_h = [Buf(stage[i].ap[:, 0:3]) for i in range(3)]
        stage_d = [Buf(stage[i].ap[:, 3:TT + 3]) for i in range(3)]
        halo_all = sb("halo", [128, 24, 3], BF16)
        halo = [Buf(halo_all.ap[:, j, :]) for j in range(24)]
        fhalo_all = sb("fhalo", [128, 32, 2], BF16)
        fhalo = [Buf(fhalo_all.ap[:, j, :]) for j in range(32)]
        dtacs = sb("dtacs", [64, TT], F32)
        state = sb("state", [128, 2048], F32)
        stbf = sb("stbf", [128, 2048], BF16)
        state_h = [Buf(state.ap[:, h_ * 64:(h_ + 1) * 64]) for h_ in range(32)]
        stbf_g = [Buf(stbf.ap[:, g_ * 512:(g_ + 1) * 512]) for g_ in range(4)]
        kmT = sb("kmT", [128, 8, 32], BF16)
        ksum = sb("ksum", [128, 2], F32)
        NKB = 3
        Kt = [sb("Kt%d" % i, [128, KU], BF16) for i in range(NKB)]
        Kt_k = [Buf(Kt[i].ap[0:64, :]) for i in range(NKB)]
        Kt_a = [Buf(Kt[i].ap[64:97, :]) for i in range(NKB)]
        Vt = [sb("Vt%d" % i, [128, NSU, 65], BF16) for i in range(NKB)]
        rbufs = [sb("rbuf%d" % i, [128, D], F32) for i in range(2)]
        tm = sb("tm", [128, 64], F32)
        elast = sb("elast", [128, 32], F32)
        wend = sb("wend", [128, 32], F32)
        xdt = [sb("xdt%d" % i, [128, 512], BF16) for i in range(2)]
        xdtw = [sb("xdtw%d" % i, [128, 512], BF16) for i in range(2)]
        Btm = [sb("Btm%d" % i, [128, 128], BF16) for i in range(2)]
        cbm = sb("cbm", [128, 4, 128], F32)
        t2b = [sb("t2b%d" % i, [128, 512], BF16) for i in range(2)]
        e2b = [sb("e2b%d" % i, [128, 512], BF16) for i in range(2)]
        Mhb = [sb("Mhb%d" % i, [128, 512], BF16) for i in range(3)]
        Cpb = [sb("Cpb%d" % i, [128, 512], BF16) for i in range(3)]
        Qaug = [sb("Qaug%d" % i, [128, TT], BF16) for i in range(2)]
        gstage = sb("gstage", [128, 4, 97], BF16)
        gm = sb("gm", [128, 4, 32], F32)
        m8 = sb("m8", [128, 4, 8], F32)
        selb = sb("selb", [128, 4, 32], F32)
        vbt = sb("vbt", [128, 4, 32], F32)
        v01t = sb("v01t", [128, 4, 32], F32)
        ownt = sb("ownt", [128, 4, 32], F32)
        pT = [sb("pT%d" % i, [128, TT], BF16) for i in range(2)]
        pm = [ps("pm%d" % i, [128, 512]) for i in range(4)]
        pa0 = ps("pa0", [128, 512])
        pa1 = ps("pa1", [128, 512])
        pt1 = ps("pt1", [128, 512])
        pt0 = ps("pt0", [128, 1024], BF16)
        pt0a, pt0b = Buf(pt0.ap[:, 0:512], root=pt0), Buf(pt0.ap[:, 512:1024], root=pt0)
        pt1bf = Buf(pt1.ap[:, :].bitcast(BF16)[:, 0:512], root=pt1)
        pa0a, pa0b, pa0c = Buf(pa0.ap[:, 0:64], root=pa0), Buf(pa0.ap[:, 64:96], root=pa0), Buf(pa0.ap[:, 128:256], root=pa0)
        bcq = [Buf(pm[i].ap[:, 0:128], root=pm[i]) for i in range(2)]

        E(DVE, lambda e: e.memset(halo_all.ap[:], 0.0), writes=halo)
        E(POOL, lambda e: e.memset(Hall.ap[:], 0.0), writes=yT)
        E(POOL, lambda e: e.memset(attall.ap[:], 0.0), writes=attT)
        E(DVE, lambda e: e.memset(fhalo_all.ap[:], 0.0), writes=fhalo)
        E(DVE, lambda e: e.memset(state.ap[:], 0.0), writes=state_h)
        E(DVE, lambda e: e.memset(stbf.ap[:], 0.0), writes=stbf_g)
        E(DVE, lambda e: e.memset(gstage.ap[:], 0.0), writes=[gstage])
        E(DVE, lambda e: e.memset(kmT.ap[:], 0.0), writes=[kmT])
        for i in range(NKB):
            E(DVE, lambda e, i=i: e.memset(Vt[i].ap[:], 1.0), writes=[Vt[i]])
            E(DVE, lambda e, i=i: e.memset(Kt[i].ap[:], 0.0), writes=[Kt_k[i], Kt_a[i]])

        rr = {"pm": 0, "wb": 0, "ft": 0, "ku": 0, "pt": 0, "bq": 0, "stg": 0, "f8": 0}
        hs_ap = [rbufs[s_ // 2].ap[:, :].bitcast(BF16)[:, (s_ % 2) * D:(s_ % 2 + 1) * D] for s_ in range(4)]
        hs_b = [rbufs[s_ // 2] for s_ in range(4)]
        KsB, VsB = {}, {}
        dbg_final = []

        def dump(name, buf, shape, dt, tt_, bufs=None):
            if name not in dbg or tt_ != dbg_tile:
                return
            d_ = nc.dram_tensor("dbg_" + name, list(shape), dt, kind="ExternalOutput").ap()
            dbg_out[name] = d_
            dbg_final.append(dma(SP, d_, buf.ap[:], bufs if bufs is not None else [buf], [Buf(None)]))

        def nxt(key, lst):
            i = rr[key]
            rr[key] = i + 1
            return lst[i % len(lst)]

        cast_jobs = []

        def add_cast(name, wsrc, wdst, r0, r1, c0, c1):
            b = Buf(None, "wb_%s_%d_%d" % (name, r0, c0))
            wbB.setdefault(name, []).append((r0, r1, c0, c1, b))
            cast_jobs.append((b, wdst[r0:r1, c0:c1], wsrc[r0:r1, c0:c1]))

        def cast_cols(name, wsrc, wdst, nrows, c0, c1, step=512):
            c = c0
            while c < c1:
                add_cast(name, wsrc, wdst, 0, nrows, c, min(c + step, c1))
                c += step

        cast_cols("in", w_in, wb_in, D, OFF_XBC, OFF_XBC + 2560)
        cast_cols("in", w_in, wb_in, D, OFF_DT, OFF_DT + 32)
        cast_cols("in", w_in, wb_in, D, OFF_K, OFF_K + 2048)
        n_cast_first = len(cast_jobs)
        cast_cols("in", w_in, wb_in, D, OFF_XBC + 2560, OFF_XBC + 3072)
        cast_cols("in", w_in, wb_in, D, OFF_Q, OFF_Q + 1024)
        cast_cols("in", w_in, wb_in, D, OFF_Z, OFF_Z + 2048)
        cast_cols("in", w_in, wb_in, D, OFF_GS, OFF_GS + 2048)
        for r in range(0, 2048, 512):
            add_cast("ssd", w_ssd, wb_ssd, r, r + 512, 0, D)
        for r in range(0, D, 512):
            add_cast("attn", w_attn, wb_attn, r, r + 512, 0, D)
        for r in range(0, D, 512):
            add_cast("out", w_out, wb_out, r, r + 512, 0, D)
        cast_cols("up", w_up, wb_up, D, 0, 8192)
        for r in range(0, 4096, 512):
            add_cast("down", w_down, wb_down, r, r + 512, 0, D)
        cast_state = {"i": 0}

        def issue_casts(n):
            while n > 0 and cast_state["i"] < len(cast_jobs):
                b, o, i_ = cast_jobs[cast_state["i"]]
                cast_state["i"] += 1
                dma(POOL, o, i_, [DIN], [b])
                n -= 1

        def wdeps(name, r0, r1, c0, c1):
            res = []
            for (a0, a1, b0, b1, b) in wbB[name]:
                if a0 < r1 and r0 < a1 and b0 < c1 and c0 < b1:
                    res.append(b)
            assert res, (name, r0, r1, c0, c1)
            return res

        def load_panel(name, wdram, KC, c0, ncols, dup=False):
            wbf = nxt("wb", wbufs)
            PW = 4096 // KC
            view = wbf.ap[:, :].rearrange("p (c n) -> p c n", c=KC)
            src = wdram[:, c0:c0 + ncols].rearrange("(c p) n -> p c n", p=128)
            deps = wdeps(name, 0, KC * 128, c0, c0 + ncols)
            dma(SP, view[:, :, 0:ncols], src, deps, [wbf])
            if dup:
                dma(SP, view[:, :, ncols:2 * ncols], src, deps, [wbf])
            return wbf, view

        def proj_fm(name, wdram, KC, c0, ntiles, srcT, consume, ts=0):
            ptiles = (4096 // KC) // 128
            pend = []
            for p0 in range(0, ntiles, ptiles):
                npan = min(ptiles, ntiles - p0)
                wbf, view = load_panel(name, wdram, KC, c0 + p0 * 128, npan * 128)
                for j in range(npan):
                    pmb = nxt("pm", pm)
                    for c in range(KC):
                        E(PE, lambda e, c=c, j=j, pmb=pmb, view=view: e.matmul(pmb.ap[:, ts:TT], lhsT=view[:, c, j * 128:(j + 1) * 128], rhs=srcT[c][:, ts:TT],
                                                                                  start=(c == 0), stop=(c == KC - 1)),
                          reads=[wbf, (srcT.bufs[c] if len(srcT.bufs) > 1 else srcT.bufs[0])], writes=[pmb])
                    stages = consume(p0 + j, pmb)
                    if stages:
                        stages[0]()
                        for rest in reversed(pend):
                            if rest:
                                rest.pop(0)()
                        pend.append(list(stages[1:]))
            while any(pend):
                for rest in reversed(pend):
                    if rest:
                        rest.pop(0)()

        def srcT_bufs(srcT):
            return srcT.bufs

        class Src:
            def __init__(self, aps, bufs):
                self.aps, self.bufs = aps, bufs

            def __getitem__(self, c):
                return self.aps[c]

        def rms_rstd(src_ap_fn, src_bufs, n):
            for s in range(4):
                E(ACT, lambda e, s=s: e.activation(out=junk.ap[:, 0:n], in_=src_ap_fn(s), func=AF.Square, accum_out=ssq.ap[:, s:s + 1]),
                  reads=([src_bufs[s]] if len(src_bufs) == 4 else src_bufs), writes=[junk, ssq])
            E(ACT, lambda e: e.activation(out=rstd.ap[:], in_=ssq.ap[:], func=AF.Ln, scale=1.0 / n, bias=EPS), reads=[ssq], writes=[rstd])
            E(ACT, lambda e: e.activation(out=rstd.ap[:], in_=rstd.ap[:], func=AF.Exp, scale=-0.5), reads=[rstd], writes=[rstd])

        def norm_transpose(gain_fm):
            rms_rstd(lambda s: xt.ap[:, s, :], xt_s, D)
            for s in range(4):
                E(DVE, lambda e, s=s: e.tensor_scalar(out=hs_ap[s], in0=xt.ap[:, s, :], scalar1=rstd.ap[:, s:s + 1], scalar2=None, op0=ALU.mult),
                  reads=[xt_s[s], rstd], writes=[hs_b[s]], same_ok=True)
            for c in range(8):
                ptb = pt0a if c % 2 == 0 else pt1bf
                for s in range(4):
                    E(PE, lambda e, c=c, s=s, ptb=ptb: e.transpose(out=ptb.ap[:, s * 128:(s + 1) * 128], in_=hs_ap[s][:, c * 128:(c + 1) * 128], identity=identb.ap[:]),
                      reads=[hs_b[s], identb], writes=[ptb])
                eng = DVE if c % 2 == 0 else ACT
                if eng == DVE:
                    E(DVE, lambda e, c=c, ptb=ptb: e.tensor_scalar(out=hT.ap[:, c, :], in0=ptb.ap[:, :], scalar1=gain_fm.ap[:, c:c + 1], scalar2=None, op0=ALU.mult),
                      reads=[ptb, gain_fm], writes=[hT], same_ok=True)
                else:
                    E(ACT, lambda e, c=c, ptb=ptb: e.mul(out=hT.ap[:, c, :], in_=ptb.ap[:, :], mul=gain_fm.ap[:, c:c + 1]),
                      reads=[ptb, gain_fm], writes=[hT], same_ok=True)

        hT_src = Src([hT.ap[:, c, :] for c in range(8)], [hT])

        final_ops = []
        for tt in range(NP + NM):
            is_main = tt >= NP
            mixfull = tt >= NP - 1
            TS = (384 if SEMI_SHORT else 0) if (NP > 0 and tt == NP - 1) else 0
            SR = range(TS // 128, 4)
            tm_i = tt - NP
            tok0 = tt * TT
            issue_casts(10 ** 9 if tt >= NP - 1 else (n_cast_first if tt == 0 else 8))
            for s_ in range(4):
                dma(SP, xt.ap[:, s_, :], xw[tok0 + s_ * 128:tok0 + (s_ + 1) * 128, :], [DIN], [xt_s[s_]])
            norm_transpose(gpm)

            def conv_consume(j, pmb, base=0):
                jj = base + j
                si = rr["stg"] % 3
                rr["stg"] += 1
                stg, sh, sd = stage[si], stage_h[si], stage_d[si]
                tmp = nxt("ft", fT)

                def s0():
                    E(POOL, lambda e: e.tensor_copy(out=stg.ap[:, 0:3], in_=halo[jj].ap), reads=[halo[jj]], writes=[sh])
                    E(ACT, lambda e: e.activation(out=stg.ap[:, 3:TT + 3], in_=pmb.ap[:, :], func=AF.Copy), reads=[pmb], writes=[sd])
                    E(POOL, lambda e: e.tensor_copy(out=halo[jj].ap, in_=stg.ap[:, TT:TT + 3]), reads=[sd], writes=[halo[jj]])
                    E(ACT, lambda e: e.activation(out=tmp.ap[:, :], in_=stg.ap[:, 0:TT], func=AF.Identity, scale=cw.ap[:, jj, 0:1], bias=cb.ap[:, jj:jj + 1]),
                      reads=[sh, sd, cw, cb], writes=[tmp])

                def s1():
                    for k in range(1, 4):
                        E(DVE, lambda e, k=k: e.scalar_tensor_tensor(out=tmp.ap[:, :], in0=stg.ap[:, k:k + TT], scalar=cw.ap[:, jj, k:k + 1], in1=tmp.ap[:, :], op0=ALU.mult, op1=ALU.add),
                          reads=[sh, sd, cw, tmp], writes=[tmp])

                def s2():
                    E(ACT, lambda e: e.activation(out=xbc[jj].ap, in_=tmp.ap[:, :], func=AF.Silu), reads=[tmp], writes=[xbc[jj]])

                return [s0, s1, s2]

            nxbc = 24 if mixfull else 20
            proj_fm("in", wb_in, 8, OFF_XBC, nxbc, hT_src, conv_consume)

            wbf, view = load_panel("in", wb_in, 8, OFF_DT, 32, dup=True)
            pmb = nxt("pm", pm)
            dttmp = nxt("ft", fT)
            for c in range(8):
                E(PE, lambda e, c=c, pmb=pmb, view=view: e.matmul(pmb.ap[0:64, :], lhsT=view[:, c, 0:64], rhs=hT.ap[:, c, :], start=(c == 0), stop=(c == 7)),
                  reads=[wbf, hT], writes=[pmb])
            E(ACT, lambda e, pmb=pmb, dttmp=dttmp: e.activation(out=dttmp.ap[0:64, :], in_=pmb.ap[0:64, :], func=AF.Exp, bias=dtb.ap[:, 0:1]), reads=[pmb, dtb], writes=[dttmp])
            E(ACT, lambda e, dttmp=dttmp: e.activation(out=dtacs.ap[:, :], in_=dttmp.ap[0:64, :], func=AF.Ln, bias=1.0), reads=[dttmp], writes=[dtacs])
            E(DVE, lambda e, dttmp=dttmp: e.tensor_scalar(out=dttmp.ap[32:64, :], in0=dtacs.ap[32:64, :], scalar1=aneg.ap[32:64, 0:1], scalar2=None, op0=ALU.mult),
              reads=[dtacs, aneg], writes=[dttmp])
            for ci in range(4):
                E(DVE, lambda e, ci=ci, dttmp=dttmp: e.tensor_tensor_scan(out=dtacs.ap[32:64, ci * 128:(ci + 1) * 128], data0=onesf.ap[32:64, :],
                                                              data1=dttmp.ap[32:64, ci * 128:(ci + 1) * 128], initial=0.0, op0=ALU.mult, op1=ALU.add),
                  reads=[dttmp, onesf], writes=[dtacs], same_ok=(ci > 0))
            dump("dtacs", dtacs, [64, TT], F32, tt)

            KsB[tt] = Buf(None, "Ks%d" % tt)
            VsB[tt] = Buf(None, "Vs%d" % tt)

            def k_consume(j, pmb):
                kb = yT[8 + j % 2]
                E(ACT, lambda e: e.activation(out=kb.ap, in_=pmb.ap[:, :], func=AF.Copy), reads=[pmb], writes=[kb])
                E(DVE, lambda e: e.tensor_reduce(out=ksum.ap[:, :], in_=kb.ap.rearrange("p (b t) -> p b t", b=2), axis=AX.X, op=ALU.add),
                  reads=[kb], writes=[ksum])
                km_ap = kmT.ap[:, j, 2 * tt:2 * tt + 2]
                E(DVE, lambda e: e.tensor_scalar(out=km_ap, in0=ksum.ap[:, :], scalar1=1.0 / 256, scalar2=None, op0=ALU.mult),
                  reads=[ksum], writes=[kmT])
                dma(SP, Ks[2 * j:2 * j + 2, :, tok0:tok0 + TT].rearrange("h r t -> (h r) t"), kb.ap, [kb], [KsB[tt]])

            proj_fm("in", wb_in, 8, OFF_K, 8, hT_src, k_consume)

            wv = [load_panel("in", wb_in, 8, OFF_V + hf * 512, 512) for hf in range(2)]
            for s in range(4):
                vi = 10 + 2 * (s % 2)
                vb_bufs = [yT[vi], yT[vi + 1]]
                vb_ap = Hall.ap[:, vi:vi + 2, :].rearrange("p a t -> p (a t)")
                for hf in range(2):
                    wbf, view = wv[hf]
                    pmb = nxt("pm", pm)
                    for c in range(8):
                        E(PE, lambda e, c=c, s=s, pmb=pmb, view=view: e.matmul(pmb.ap[:, :], lhsT=hT.ap[:, c, s * 128:(s + 1) * 128], rhs=view[:, c, 0:512],
                                                                                  start=(c == 0), stop=(c == 7)),
                          reads=[wbf, hT], writes=[pmb])
                    if hf == 0:
                        E(DVE, lambda e, pmb=pmb, vb_ap=vb_ap: e.tensor_copy(out=vb_ap[:, 0:512], in_=pmb.ap[:, :]), reads=[pmb], writes=[vb_bufs[0]])
                    else:
                        E(ACT, lambda e, pmb=pmb, vb_ap=vb_ap: e.activation(out=vb_ap[:, 512:1024], in_=pmb.ap[:, :], func=AF.Copy), reads=[pmb], writes=[vb_bufs[1]])
                dma(SP, Vs[:, :, tt * 4 + s, :].rearrange("h p d -> p h d"), vb_ap.rearrange("p (h d) -> p h d", h=16), vb_bufs, [VsB[tt]])

            def ssd_chunk(ci):
                c0, c1 = ci * 128, (ci + 1) * 128
                doy = mixfull and c0 >= TS

                def C_pro():
                    E(PE, lambda e: e.matmul(pa0a.ap[:, 0:64], lhsT=dtacs.ap[0:64, c0:c1], rhs=identf.ap[:, :], start=True, stop=True),
                      reads=[dtacs, identf], writes=[pa0a])
                    E(DVE, lambda e: e.tensor_copy(out=tm.ap[:, :], in_=pa0a.ap[:, 0:64]), reads=[pa0a], writes=[tm])
                    E(PE, lambda e: e.matmul(pa0b.ap[:, 0:32], lhsT=dtacs.ap[0:64, c1 - 1:c1].to_broadcast([64, 128]), rhs=selcol.ap[:, :], start=True, stop=True),
                      reads=[dtacs, selcol], writes=[pa0b])
                    E(ACT, lambda e: e.activation(out=elast.ap[:, :], in_=pa0b.ap[:, 0:32], func=AF.Exp), reads=[pa0b], writes=[elast])
                    E(DVE, lambda e: e.tensor_tensor(out=wend.ap[:, :], in0=pa0b.ap[:, 0:32], in1=tm.ap[:, 32:64], op=ALU.subtract), reads=[pa0b, tm], writes=[wend])
                    E(ACT, lambda e: e.activation(out=wend.ap[:, :], in_=wend.ap[:, :], func=AF.Exp), reads=[wend], writes=[wend])
                    if doy:
                        for g in range(4):
                            E(PE, lambda e, g=g: e.matmul(pa1.ap[:, g * 128:(g + 1) * 128], lhsT=xbc[16 + g].ap[:, c0:c1], rhs=xbc[20 + g].ap[:, c0:c1], start=True, stop=True),
                              reads=[xbc[16 + g], xbc[20 + g]], writes=[pa1])
                        E(DVE, lambda e: e.tensor_tensor(out=cbm.ap[:, :, :], in0=pa1.ap[:, :].rearrange("p (g l) -> p g l", g=4),
                                                         in1=triu.ap[:, :].unsqueeze(1).to_broadcast([128, 4, 128]), op=ALU.mult),
                          reads=[pa1, triu], writes=[cbm])

                def G_pro1(g):
                    for k in range(4):
                        E(PE, lambda e, k=k: e.transpose(out=pt0a.ap[:, k * 128:(k + 1) * 128], in_=xbc[4 * g + k].ap[:, c0:c1], identity=identb.ap[:]),
                          reads=[xbc[4 * g + k], identb], writes=[pt0a])
                    E(PE, lambda e: e.transpose(out=pt0b.ap[:, 0:128], in_=xbc[16 + g].ap[:, c0:c1], identity=identb.ap[:]),
                      reads=[xbc[16 + g], identb], writes=[pt0b])
                    xd, xdw, bt = xdt[g % 2], xdtw[g % 2], Btm[g % 2]
                    E(DVE, lambda e: e.tensor_tensor(out=xd.ap[:, :].rearrange("p (h d) -> p h d", h=8), in0=pt0a.ap[:, :].rearrange("p (h d) -> p h d", h=8),
                                                     in1=tm.ap[:, 8 * g:8 * g + 8].unsqueeze(2).to_broadcast([128, 8, 64]), op=ALU.mult),
                      reads=[pt0a, tm], writes=[xd])
                    E(POOL, lambda e: e.tensor_tensor(out=xdw.ap[:, :].rearrange("p (h d) -> p h d", h=8), in0=xd.ap[:, :].rearrange("p (h d) -> p h d", h=8),
                                                      in1=wend.ap[:, 8 * g:8 * g + 8].unsqueeze(2).to_broadcast([128, 8, 64]), op=ALU.mult),
                      reads=[xd, wend], writes=[xdw])
                    E(ACT, lambda e: e.activation(out=bt.ap[:, :], in_=pt0b.ap[:, 0:128], func=AF.Copy), reads=[pt0b], writes=[bt])

                def dS(g):
                    xdw, bt = xdtw[g % 2], Btm[g % 2]
                    E(PE, lambda e: e.matmul(pt1.ap[:, :], lhsT=bt.ap[:, :], rhs=xdw.ap[:, :], start=True, stop=True), reads=[bt, xdw], writes=[pt1])

                def A(g, q4, bi):
                    h0 = 8 * g + 4 * q4
                    bqb = pm[bi % 2]
                    a1 = nxt("ft", fT)
                    a2, ee = t2b[bi % 2], e2b[bi % 2]
                    mh, cp = Mhb[bi % 3], Cpb[bi % 3]
                    for i4 in range(4):
                        E(PE, lambda e, h=h0 + i4, i4=i4: e.matmul(bqb.ap[:, i4 * 128:(i4 + 1) * 128], lhsT=selcol.ap[:, h:h + 1].to_broadcast([64, 128]),
                                                                   rhs=dtacs.ap[0:64, c0:c1], start=True, stop=True),
                          reads=[selcol, dtacs], writes=[bqb])
                    E(DVE, lambda e: e.tensor_tensor(out=a1.ap[:, :].rearrange("p (h l) -> p h l", h=4), in0=bqb.ap[:, :].rearrange("p (h l) -> p h l", h=4),
                                                     in1=tm.ap[:, 32 + h0:36 + h0].unsqueeze(2).to_broadcast([128, 4, 128]), op=ALU.subtract),
                      reads=[bqb, tm], writes=[a1])
                    E(DVE, lambda e: e.tensor_scalar(out=a1.ap[:, :], in0=a1.ap[:, :], scalar1=0.0, scalar2=None, op0=ALU.min), reads=[a1], writes=[a1])
                    E(ACT, lambda e: e.activation(out=a2.ap[:, :], in_=a1.ap[:, :], func=AF.Exp), reads=[a1], writes=[a2])
                    E(ACT, lambda e: e.activation(out=ee.ap[:, :], in_=bqb.ap[:, :], func=AF.Exp), reads=[bqb], writes=[ee])
                    E(DVE, lambda e: e.scalar_tensor_tensor(out=mh.ap[:, :].rearrange("p (h l) -> p h l", h=4), in0=a2.ap[:, :].rearrange("p (h l) -> p h l", h=4), scalar=1.0,
                                                            in1=cbm.ap[:, g, :].unsqueeze(1).to_broadcast([128, 4, 128]), op0=ALU.min, op1=ALU.mult),
                      reads=[a2, cbm], writes=[mh])
                    E(POOL, lambda e: e.tensor_tensor(out=cp.ap[:, :].rearrange("p (h l) -> p h l", h=4), in0=ee.ap[:, :].rearrange("p (h l) -> p h l", h=4),
                                                      in1=xbc[20 + g].ap[:, c0:c1].unsqueeze(1).to_broadcast([128, 4, 128]), op=ALU.mult),
                      reads=[ee, xbc[20 + g]], writes=[cp])

                def B(g, q4, bi):
                    ypm = pm[2 + g % 2]
                    xd = xdt[g % 2]
                    mh, cp = Mhb[bi % 3], Cpb[bi % 3]
                    for i4 in range(4):
                        hl = 4 * q4 + i4
                        h = 8 * g + hl
                        po = ypm.ap[(hl % 2) * 64:(hl % 2) * 64 + 64, (hl // 2) * 128:(hl // 2 + 1) * 128]
                        E(PE, lambda e, po=po, hl=hl, i4=i4: e.matmul(po, lhsT=xd.ap[:, hl * 64:(hl + 1) * 64], rhs=mh.ap[:, i4 * 128:(i4 + 1) * 128], start=True, stop=False),
                          reads=[xd, mh], writes=[ypm])
                        E(PE, lambda e, po=po, h=h, i4=i4: e.matmul(po, lhsT=stbf.ap[:, h * 64:(h + 1) * 64], rhs=cp.ap[:, i4 * 128:(i4 + 1) * 128], start=False, stop=True),
                          reads=[stbf_g[g], cp], writes=[ypm])

                def G_epi(g):
                    if doy:
                        ypm = pm[2 + g % 2]
                        for k in range(4):
                            ct = 4 * g + k
                            E(DVE, lambda e, ct=ct, k=k: e.scalar_tensor_tensor(out=yT[ct].ap[:, c0:c1], in0=xbc[ct].ap[:, c0:c1], scalar=dsk.ap[:, ct:ct + 1],
                                                                                in1=ypm.ap[:, k * 128:(k + 1) * 128], op0=ALU.mult, op1=ALU.add),
                              reads=[xbc[ct], dsk, ypm], writes=[yT[ct]], same_ok=True)
                    stg_b = state_h[8 * g:8 * g + 8]
                    E(DVE, lambda e: e.tensor_tensor(out=state.ap[:, g * 512:(g + 1) * 512].rearrange("p (h d) -> p h d", h=8), in0=state.ap[:, g * 512:(g + 1) * 512].rearrange("p (h d) -> p h d", h=8),
                                                     in1=elast.ap[:, 8 * g:8 * g + 8].unsqueeze(2).to_broadcast([128, 8, 64]), op=ALU.mult),
                      reads=stg_b + [elast], writes=stg_b)
                    E(DVE, lambda e: e.tensor_tensor(out=state.ap[:, g * 512:(g + 1) * 512], in0=state.ap[:, g * 512:(g + 1) * 512], in1=pt1.ap[:, :], op=ALU.add),
                      reads=stg_b + [pt1], writes=stg_b)
                    E(ACT, lambda e: e.activation(out=stbf.ap[:, g * 512:(g + 1) * 512], in_=state.ap[:, g * 512:(g + 1) * 512], func=AF.Copy), reads=stg_b, writes=[stbf_g[g]])

                C_pro()
                G_pro1(0)
                if not doy:
                    for g in range(4):
                        if g + 1 < 4:
                            G_pro1(g + 1)
                        dS(g)
                        G_epi(g)
                    return
                batches = [(g, q4) for g in range(4) for q4 in range(2)]
                dS(0)
                A(0, 0, 0)
                A(0, 1, 1)
                for bi, (g, q4) in enumerate(batches):
                    if bi + 2 < len(batches):
                        g2, q42 = batches[bi + 2]
                        if q42 == 0:
                            G_pro1(g2)
                        A(g2, q42, bi + 2)
                    B(g, q4, bi)
                    if q4 == 1:
                        G_epi(g)
                        if g + 1 < 4:
                            dS(g + 1)

            for ci in range(4):
                ssd_chunk(ci)
            if NP > 0 and tt == NP - 1:
                E(DVE, lambda e: e.tensor_scalar(out=state.ap[:, :], in0=state.ap[:, :], scalar1=pv.ap[:, 0:1], scalar2=None, op0=ALU.mult), reads=state_h + [pv], writes=state_h)
                E(ACT, lambda e: e.activation(out=stbf.ap[:, :], in_=state.ap[:, :], func=AF.Copy), reads=state_h, writes=stbf_g)
            if not mixfull:
                continue
            for ct in (0, 5, 15):
                dump("yraw%d" % ct, yT[ct], [128, TT], BF16, tt)

            def q_consume(j, pmb):
                E(ACT, lambda e: e.activation(out=qT[j].ap, in_=pmb.ap[:, :], func=AF.Copy), reads=[pmb], writes=[qT[j]])

            proj_fm("in", wb_in, 8, OFF_Q, 8, hT_src, q_consume, ts=TS)

            a_i = tt - (NP - 1) if NP > 0 else tt + 1
            for s in range(4):
                o = 2 * a_i + s // 2
                E(POOL, lambda e, s=s, o=o: e.tensor_copy(out=vbt.ap[:, s, :], in_=vtab.ap[:, o, :]), reads=[vtab], writes=[vbt], same_ok=True)
                E(POOL, lambda e, s=s, o=o: e.tensor_scalar(out=v01t.ap[:, s, :], in0=vtab.ap[:, o, :], scalar1=-1.0, scalar2=None, op0=ALU.is_ge), reads=[vtab], writes=[v01t], same_ok=True)
                E(POOL, lambda e, s=s, o=o: e.tensor_copy(out=ownt.ap[:, s, :], in_=own01.ap[:, o, :]), reads=[own01], writes=[ownt], same_ok=True)
            nsub_total = (tt + 1) * 4
            nun = (nsub_total + NSU - 1) // NSU
            def keep_sub(h, ksi):
                if ksi >= tt * 4:
                    return True
                return SLOPES[h] * (tt * TT - (ksi * 128 + 127)) <= ALIBI_CUT

            head_subs = [[(u, ks) for u in range(nun) for ks in range(min(NSU, nsub_total - u * NSU)) if keep_sub(h, u * NSU + ks)] for h in range(16)]
            units = [(h, u) for h in range(16) for u in sorted(set(u for u, _ in head_subs[h]))]
            unit_idx = {hu: j for j, hu in enumerate(units)}
            unit_buf = {}

            def load_unit(j):
                if j >= len(units) or j in unit_buf:
                    return
                h, u = units[j]
                bi_ = rr["ku"] % NKB
                kt, vt = Kt[bi_], Vt[bi_]
                rr["ku"] += 1
                ns = min(NSU, nsub_total - u * NSU)
                k0 = u * KU
                tl = sorted(set((k0 + i * 128) // TT for i in range(ns)))
                dma(SP, kt.ap[0:64, 0:ns * 128], Ks[h, :, k0:k0 + ns * 128], [KsB[t_] for t_ in tl], [Kt_k[bi_]])
                dma(SP, kt.ap[64:97, 0:ns * 128], kaug_d[:, k0:k0 + ns * 128], [DIN], [Kt_a[bi_]])
                dma(SP, vt.ap[:, 0:ns, 0:64], Vs[h, :, u * NSU:u * NSU + ns, :], [VsB[t_] for t_ in tl], [vt])
                unit_buf[j] = (kt, vt, [Kt_k[bi_], Kt_a[bi_]])

            def prep_head(h):
                hp, pb = h // 2, 64 * (h % 2)
                qa = Qaug[h % 2]
                dma(SP, qa.ap[0:64, :], qT[hp].ap[pb:pb + 64, :], [qT[hp]], [qa])
                for s in SR:
                    E(PE, lambda e, s=s, hp=hp, pb=pb: e.matmul(pa0c.ap[:, s * 32:(s + 1) * 32], lhsT=qT[hp].ap[pb:pb + 64, s * 128:(s + 1) * 128], rhs=kmT.ap[pb:pb + 64, hp, :],
                                                                start=True, stop=True),
                      reads=[qT[hp], kmT], writes=[pa0c])
                S0 = TS // 128
                E(DVE, lambda e: e.tensor_tensor(out=gm.ap[:, S0:4, :], in0=pa0c.ap[:, :].rearrange("p (s n) -> p s n", s=4)[:, S0:4, :], in1=vbt.ap[:, S0:4, :], op=ALU.add),
                  reads=[pa0c, vbt], writes=[gm])
                for s in SR:
                    E(DVE, lambda e, s=s: e.max(out=m8.ap[:, s, :], in_=gm.ap[:, s, :]), reads=[gm], writes=[m8], same_ok=(s > SR[0]))
                E(DVE, lambda e: e.tensor_tensor(out=selb.ap[:, S0:4, :], in0=gm.ap[:, S0:4, :], in1=m8.ap[:, S0:4, 2:3].to_broadcast([128, 4 - S0, 32]), op=ALU.is_ge),
                  reads=[gm, m8], writes=[selb])
                E(DVE, lambda e: e.tensor_tensor(out=selb.ap[:, S0:4, :], in0=selb.ap[:, S0:4, :], in1=v01t.ap[:, S0:4, :], op=ALU.mult), reads=[selb, v01t], writes=[selb])
                E(DVE, lambda e: e.tensor_tensor(out=selb.ap[:, S0:4, :], in0=selb.ap[:, S0:4, :], in1=ownt.ap[:, S0:4, :], op=ALU.add), reads=[selb, ownt], writes=[selb])
                E(DVE, lambda e: e.tensor_scalar(out=gstage.ap[:, S0:4, 64:96], in0=selb.ap[:, S0:4, :], scalar1=BIG, scalar2=-BIG, op0=ALU.mult, op1=ALU.add),
                  reads=[selb], writes=[gstage])
                E(DVE, lambda e, h=h: e.tensor_copy(out=gstage.ap[:, :, 96:97], in_=qsh.ap[:, h, :].unsqueeze(2)), reads=[qsh], writes=[gstage])
                if h == 3:
                    dump("selb", selb, [128, 4, 32], F32, tt)
                    dump("gm", gm, [128, 4, 32], F32, tt)
                for s in SR:
                    E(PE, lambda e, s=s: e.transpose(out=pt0a.ap[0:97, s * 128:(s + 1) * 128], in_=gstage.ap[:, s, :], identity=identb.ap[:]),
                      reads=[gstage, identb], writes=[pt0a])
                E(ACT, lambda e, TS=TS, qa=qa: e.activation(out=qa.ap[64:97, TS:TT], in_=pt0a.ap[64:97, TS:TT], func=AF.Copy), reads=[pt0a], writes=[qa])

            LOOK = 2
            spm = pm[0:3]
            load_unit(0)
            load_unit(1)
            load_unit(2)
            prep_head(0)
            def z_consume(j, pmb):
                tmp = nxt("ft", fT)
                si = rr["stg"] % 3
                rr["stg"] += 1
                sqb = stage[si]
                sq_bufs = [stage_h[si], stage_d[si]]

                def s0():
                    E(ACT, lambda e: e.activation(out=tmp.ap[:, :], in_=pmb.ap[:, :], func=AF.Silu), reads=[pmb], writes=[tmp])
                    E(DVE, lambda e: e.tensor_tensor(out=yT[j].ap, in0=yT[j].ap, in1=tmp.ap[:, :], op=ALU.mult), reads=[yT[j], tmp], writes=[yT[j]])
                    E(POOL, lambda e: e.tensor_tensor(out=sqb.ap[:, 0:TT], in0=yT[j].ap, in1=yT[j].ap, op=ALU.mult), reads=[yT[j]], writes=sq_bufs)

                def s1():
                    E(PE, lambda e: e.matmul(pa1.ap[:, :], lhsT=onesb.ap[:, :], rhs=sqb.ap[:, 0:TT], start=(j % 4 == 0), stop=(j % 4 == 3)), reads=[onesb] + sq_bufs, writes=[pa1])
                    if j % 4 == 3:
                        rs = nxt("ft", fT)
                        E(ACT, lambda e: e.activation(out=rs.ap[:, :], in_=pa1.ap[:, :], func=AF.Ln, scale=1.0 / 512, bias=EPS), reads=[pa1], writes=[rs])
                        E(ACT, lambda e: e.activation(out=rs.ap[:, :], in_=rs.ap[:, :], func=AF.Exp, scale=-0.5), reads=[rs], writes=[rs])
                        for k in range(4):
                            ct = j - 3 + k
                            E(DVE, lambda e, ct=ct: e.scalar_tensor_tensor(out=yT[ct].ap, in0=yT[ct].ap, scalar=gon.ap[:, ct:ct + 1], in1=rs.ap[:, :], op0=ALU.mult, op1=ALU.mult),
                              reads=[yT[ct], gon, rs], writes=[yT[ct]])

                return [s0, s1]

            proj_fm("in", wb_in, 8, OFF_Z, 16, hT_src, z_consume, ts=TS)
            for ct in (0, 5, 15):
                dump("yn%d" % ct, yT[ct], [128, TT], BF16, tt)

            for h in range(16):
                qa = Qaug[h % 2]
                acc = pa1 if h % 2 == 0 else pm[3]
                subs = head_subs[h]
                n = len(subs)
                spbs = {}

                def emit_qk(i, h=h, qa=qa, subs=subs, spbs=spbs):
                    u, ks = subs[i]
                    j = unit_idx[(h, u)]
                    if j not in unit_buf or i == 0 or subs[i - 1][0] != u:
                        load_unit(j)
                        load_unit(j + 1)
                    kt, vt, ktb = unit_buf[j]
                    ksi = u * NSU + ks
                    in_tile = ksi >= tt * 4
                    spb = spm[rr["pm"] % 3]
                    rr["pm"] += 1
                    E(PE, lambda e, TS=TS, ks=ks, kt=kt, qa=qa, spb=spb, in_tile=in_tile: e.matmul(spb.ap[:, TS:TT], lhsT=kt.ap[0:97, ks * 128:(ks + 1) * 128], rhs=qa.ap[0:97, TS:TT],
                                                                                             start=True, stop=(not in_tile)),
                      reads=ktb + [qa], writes=[spb])
                    if in_tile:
                        cidx = ksi - tt * 4
                        E(PE, lambda e, TS=TS, cidx=cidx, spb=spb: e.matmul(spb.ap[:, TS:TT], lhsT=identb.ap[:, :], rhs=cmask.ap[:, cidx, TS:TT], start=False, stop=True),
                          reads=[identb, cmask], writes=[spb])
                    spbs[i] = spb

                for i in range(min(LOOK, n)):
                    emit_qk(i)
                for i in range(n):
                    if i + LOOK < n:
                        emit_qk(i + LOOK)
                    if i == min(4, n - 1) and h + 1 < 16:
                        prep_head(h + 1)
                    u, ks = subs[i]
                    kt, vt, ktb = unit_buf[unit_idx[(h, u)]]
                    ksi = u * NSU + ks
                    spb = spbs[i]
                    ptb = pT[rr["pt"] % len(pT)]
                    rr["pt"] += 1
                    off = ksi - tt * 4 + NOFFMAX
                    E(ACT, lambda e, TS=TS, spb=spb, ptb=ptb, h=h, off=off: e.activation(out=ptb.ap[:, TS:TT], in_=spb.ap[:, TS:TT], func=AF.Exp, scale=0.125, bias=abias.ap[:, h, off:off + 1]),
                      reads=[spb, abias], writes=[ptb])
                    E(PE, lambda e, TS=TS, ks=ks, vt=vt, ptb=ptb, acc=acc, st_=(i == 0), sp_=(i == n - 1): e.matmul(acc.ap[0:65, TS:TT], lhsT=vt.ap[:, ks, 0:65], rhs=ptb.ap[:, TS:TT], start=st_, stop=sp_),
                      reads=[vt, ptb], writes=[acc])
                numer, den = nxt("ft", fT), nxt("ft", fT)
                E(ACT, lambda e, TS=TS, numer=numer, acc=acc: e.activation(out=numer.ap[0:64, TS:TT], in_=acc.ap[0:64, TS:TT], func=AF.Copy), reads=[acc], writes=[numer])
                E(ACT, lambda e, TS=TS, den=den, acc=acc: e.activation(out=den.ap[64:65, TS:TT], in_=acc.ap[64:65, TS:TT], func=AF.Copy), reads=[acc], writes=[den])
                E(PE, lambda e, TS=TS, den=den: e.matmul(pt1.ap[0:64, TS:TT], lhsT=onesf.ap[64:65, 0:64], rhs=den.ap[64:65, TS:TT], start=True, stop=True), reads=[onesf, den], writes=[pt1])
                E(DVE, lambda e, TS=TS, den=den: e.reciprocal(out=den.ap[0:64, TS:TT], in_=pt1.ap[0:64, TS:TT]), reads=[pt1, den], writes=[den])
                E(DVE, lambda e, TS=TS, h=h, numer=numer, den=den: e.tensor_tensor(out=attT[h].ap[:, TS:TT], in0=numer.ap[0:64, TS:TT], in1=den.ap[0:64, TS:TT], op=ALU.mult), reads=[numer, den], writes=[attT[h]])
            for h in (0, 3, 15):
                dump("att%d" % h, attT[h], [64, TT], BF16, tt)

            sgs, sga, tmx = G[0:8], G[8:16], G[16:24]

            def gs_consume(j, pmb):
                E(ACT, lambda e: e.activation(out=sgs[j].ap, in_=pmb.ap[:, :], func=AF.Sigmoid), reads=[pmb], writes=[sgs[j]])

            def ga_consume(j, pmb):
                E(ACT, lambda e: e.activation(out=sga[j].ap, in_=pmb.ap[:, :], func=AF.Sigmoid), reads=[pmb], writes=[sga[j]])

            proj_fm("in", wb_in, 8, OFF_GS, 8, hT_src, gs_consume, ts=TS)
            proj_fm("in", wb_in, 8, OFF_GA, 8, hT_src, ga_consume, ts=TS)
            yT_src = Src([yT[c].ap for c in range(16)], yT)

            def ys_consume(j, pmb):
                E(DVE, lambda e: e.tensor_tensor(out=tmx[j].ap, in0=pmb.ap[:, :], in1=sgs[j].ap, op=ALU.mult), reads=[pmb, sgs[j]], writes=[tmx[j]])

            proj_fm("ssd", wb_ssd, 16, 0, 8, yT_src, ys_consume, ts=TS)
            mixT = yT[0:8]
            for p0 in (0, 2, 4, 6):
                wbf = nxt("wb", wbufs)
                view = wbf.ap[0:64, :].rearrange("p (c n) -> p c n", c=16)
                dma(SP, view[:, :, 0:256], wb_attn[:, p0 * 128:p0 * 128 + 256].rearrange("(h d) n -> d h n", d=64), wdeps("attn", 0, D, p0 * 128, p0 * 128 + 256), [wbf])
                for j in range(2):
                    jj = p0 + j
                    pmb = nxt("pm", pm)
                    for hh in range(16):
                        E(PE, lambda e, TS=TS, hh=hh, j=j, pmb=pmb, view=view: e.matmul(pmb.ap[:, TS:TT], lhsT=view[:, hh, j * 128:(j + 1) * 128], rhs=attT[hh].ap[:, TS:TT], start=(hh == 0), stop=(hh == 15)),
                          reads=[wbf, attT[hh]], writes=[pmb])
                    tmp = nxt("ft", fT)
                    E(DVE, lambda e, jj=jj, pmb=pmb, tmp=tmp: e.tensor_tensor(out=tmp.ap[:, :], in0=pmb.ap[:, :], in1=sga[jj].ap, op=ALU.mult), reads=[pmb, sga[jj]], writes=[tmp])
                    E(POOL, lambda e, jj=jj, tmp=tmp: e.tensor_tensor(out=mixT[jj].ap, in0=tmp.ap[:, :], in1=tmx[jj].ap, op=ALU.add), reads=[tmp, tmx[jj]], writes=[mixT[jj]])
            for ct in (0, 7):
                dump("mix%d" % ct, mixT[ct], [128, TT], BF16, tt)

            def post_norm_residual(rb, gain_bc, s, dst_ap, dst_bufs):
                E(ACT, lambda e: e.activation(out=junk.ap[:, :], in_=rb.ap[:, :], func=AF.Square, accum_out=ssq.ap[:, 0:1]), reads=[rb], writes=[junk, ssq])
                E(ACT, lambda e: e.activation(out=rstd.ap[:, 0:1], in_=ssq.ap[:, 0:1], func=AF.Ln, scale=1.0 / D, bias=EPS), reads=[ssq], writes=[rstd])
                E(ACT, lambda e: e.activation(out=rstd.ap[:, 0:1], in_=rstd.ap[:, 0:1], func=AF.Exp, scale=-0.5), reads=[rstd], writes=[rstd])
                E(DVE, lambda e: e.scalar_tensor_tensor(out=rb.ap[:, :], in0=rb.ap[:, :], scalar=rstd.ap[:, 0:1], in1=gain_bc.ap[:, :], op0=ALU.mult, op1=ALU.mult),
                  reads=[rb, rstd, gain_bc], writes=[rb])
                E(POOL, lambda e: e.tensor_tensor(out=dst_ap, in0=xt.ap[:, s, :], in1=rb.ap[:, :], op=ALU.add), reads=[xt_s[s], rb], writes=dst_bufs)

            wo = [load_panel("out", wb_out, 8, hf * 512, 512) for hf in range(2)]
            for s in SR:
                rb = rbufs[s % 2]
                for hf in range(2):
                    wbf, view = wo[hf]
                    pmb = nxt("pm", pm)
                    for c in range(8):
                        E(PE, lambda e, c=c, s=s, pmb=pmb, view=view: e.matmul(pmb.ap[:, :], lhsT=mixT[c].ap[:, s * 128:(s + 1) * 128], rhs=view[:, c, 0:512], start=(c == 0), stop=(c == 7)),
                          reads=[wbf, mixT[c]], writes=[pmb])
                    if hf == 0:
                        E(DVE, lambda e, pmb=pmb, rb=rb: e.tensor_copy(out=rb.ap[:, 0:512], in_=pmb.ap[:, :]), reads=[pmb], writes=[rb])
                    else:
                        E(ACT, lambda e, pmb=pmb, rb=rb: e.activation(out=rb.ap[:, 512:1024], in_=pmb.ap[:, :], func=AF.Copy), reads=[pmb], writes=[rb])
                post_norm_residual(rb, gqm, s, xt.ap[:, s, :], [xt_s[s]])
            dump("x1", xt, [128, 4, D], F32, tt, bufs=xt_s)

            norm_transpose(gpf)
            semi = not is_main

            ftx = [Buf(rbufs[i // 2].ap[:, (i % 2) * TT:(i % 2 + 1) * TT]) for i in range(4)]
            for a_ in ftx:
                a_.r = [o_ for p_ in rbufs for o_ in (p_.r + ([p_.w] if p_.w is not None else []))]
            fT8 = fT + ftx

            def gate_consume(j, pmb):
                si = rr["stg"] % 3
                rr["stg"] += 1
                stg, sh, sd = stage[si], stage_h[si], stage_d[si]

                def s0():
                    E(POOL, lambda e: e.tensor_copy(out=stg.ap[:, 1:3], in_=fhalo[j].ap), reads=[fhalo[j]], writes=[sh])
                    E(ACT, lambda e: e.activation(out=stg.ap[:, 3:TT + 3], in_=pmb.ap[:, :], func=AF.Copy), reads=[pmb], writes=[sd])
                    E(POOL, lambda e: e.tensor_copy(out=fhalo[j].ap, in_=stg.ap[:, TT + 1:TT + 3]), reads=[sd], writes=[fhalo[j]])

                if semi:
                    return [s0]
                tmp, x2 = fT8[rr["f8"] % 8], fT8[(rr["f8"] + 1) % 8]
                rr["f8"] += 2

                def s0b():
                    s0()
                    E(ACT, lambda e: e.activation(out=tmp.ap[:, :], in_=stg.ap[:, 1:TT + 1], func=AF.Identity, scale=fcw.ap[:, j, 0:1], bias=fcb.ap[:, j:j + 1]),
                      reads=[sh, sd, fcw, fcb], writes=[tmp])

                def s1():
                    for k in range(1, 3):
                        E(DVE, lambda e, k=k: e.scalar_tensor_tensor(out=tmp.ap[:, :], in0=stg.ap[:, 1 + k:1 + k + TT], scalar=fcw.ap[:, j, k:k + 1], in1=tmp.ap[:, :], op0=ALU.mult, op1=ALU.add),
                          reads=[sh, sd, fcw, tmp], writes=[tmp])
                    E(ACT, lambda e: e.activation(out=x2.ap[:, :], in_=tmp.ap[:, :], func=AF.Square, scale=0.21145921592590755), reads=[tmp], writes=[x2])

                def s2():
                    E(DVE, lambda e: e.scalar_tensor_tensor(out=x2.ap[:, :], in0=x2.ap[:, :], scalar=1.0, in1=tmp.ap[:, :], op0=ALU.add, op1=ALU.mult), reads=[x2, tmp], writes=[x2])
                    E(ACT, lambda e: e.activation(out=x2.ap[:, :], in_=x2.ap[:, :], func=AF.Sigmoid, scale=1.5957691216057308), reads=[x2], writes=[x2])

                def s3():
                    E(DVE, lambda e: e.tensor_tensor(out=act[j].ap, in0=tmp.ap[:, :], in1=x2.ap[:, :], op=ALU.mult), reads=[tmp, x2], writes=[act[j]])

                return [s0b, s1, s2, s3]

            proj_fm("up", wb_up, 8, 0, 32, hT_src, gate_consume, ts=TS)
            for p_ in rbufs:
                for a_ in ftx:
                    p_.r.extend(a_.r)
                    if a_.w is not None:
                        p_.r.append(a_.w)
            if semi:
                E(DVE, lambda e: e.tensor_scalar(out=fhalo_all.ap[:, :, :], in0=fhalo_all.ap[:, :, :], scalar1=pv.ap[:, 0:1], scalar2=None, op0=ALU.mult), reads=fhalo + [pv], writes=fhalo)
                continue

            def up_consume(j, pmb):
                E(DVE, lambda e: e.tensor_tensor(out=act[j].ap, in0=pmb.ap[:, :], in1=act[j].ap, op=ALU.mult), reads=[pmb, act[j]], writes=[act[j]])

            proj_fm("up", wb_up, 8, 4096, 32, hT_src, up_consume)
            for ct in (0, 31):
                dump("act%d" % ct, act[ct], [128, TT], BF16, tt)
            for sp2 in range(2):
                accs = {(s, hf): pm[(s % 2) * 2 + hf] for s in (2 * sp2, 2 * sp2 + 1) for hf in range(2)}
                for kp in range(8):
                    wbf = nxt("wb", wbufs)
                    view = wbf.ap[:, :].rearrange("p (c n) -> p c n", c=4)
                    dma(SP, view, wb_down[kp * 512:(kp + 1) * 512, :].rearrange("(c p) n -> p c n", p=128), wdeps("down", kp * 512, (kp + 1) * 512, 0, D), [wbf])
                    for (s, hf), acc in accs.items():
                        for c in range(4):
                            kc = kp * 4 + c
                            E(PE, lambda e, s=s, hf=hf, acc=acc, c=c, kc=kc, view=view: e.matmul(acc.ap[:, :], lhsT=act[kc].ap[:, s * 128:(s + 1) * 128], rhs=view[:, c, hf * 512:(hf + 1) * 512],
                                                                                                  start=(kc == 0), stop=(kc == 31)),
                              reads=[wbf, act[kc]], writes=[acc])
                for s in (2 * sp2, 2 * sp2 + 1):
                    rb = rbufs[s % 2]
                    E(DVE, lambda e, a0=accs[(s, 0)], rb=rb: e.tensor_copy(out=rb.ap[:, 0:512], in_=a0.ap[:, :]), reads=[accs[(s, 0)]], writes=[rb])
                    E(ACT, lambda e, a1_=accs[(s, 1)], rb=rb: e.activation(out=rb.ap[:, 512:1024], in_=a1_.ap[:, :], func=AF.Copy), reads=[accs[(s, 1)]], writes=[rb])
                    post_norm_residual(rb, gqf, s, rb.ap[:, :], [rb])
                    r0 = tm_i * TT + s * 128
                    final_ops.append(dma(SP, out_d[r0:r0 + 128, :], rb.ap[:, :], [rb], [Buf(None)]))
        counts = P.finalize(final_ops + dbg_final)
    return nc, counts, dbg_out


def _slopes():
    return (2.0 ** (-8.0 * np.arange(1, 17, dtype=np.float64) / 16)).astype(np.float64)


def _consts(NP, NM, second_half):
    NPB, NLB = 2 * NP, 2 * NM
    W = (NP + NM) * TT
    NOFFMAX = (NP + NM - 1) * 4
    NOFF = NOFFMAX + 4
    NUNITS = (W + KU - 1) // KU
    c = {}
    c["identb"] = np.eye(128, dtype=np.float32).astype(NPBF)
    c["identf"] = np.eye(64, dtype=np.float32)
    sel = np.zeros((64, 32), np.float32)
    sel[32 + np.arange(32), np.arange(32)] = 1.0
    c["selcol"] = sel
    c["triu"] = np.triu(np.ones((128, 128), np.float32))
    p = np.arange(128)[:, None, None]
    ci = np.arange(4)[None, :, None]
    q = np.arange(512)[None, None, :]
    c["cmask"] = np.where(ci * 128 + p > q, -BIG, 0.0).astype(np.float32).astype(NPBF)
    keys = np.arange(NUNITS * KU)
    ka = np.zeros((33, NUNITS * KU), np.float32)
    blk = keys // 256
    for r in range(32):
        ka[r, blk == r] = 1.0
    ka[32, :] = 1.0
    c["kaug"] = ka.astype(NPBF)
    sl = _slopes()
    pp = np.arange(128)[:, None, None]
    c["qsh"] = (-8.0 * sl[None, :, None] * (np.arange(4)[None, None, :] * 128 + pp)).astype(np.float32)
    c["abias"] = (sl[None, :, None] * (pp + 128.0 * (np.arange(NOFF)[None, None, :] - NOFFMAX))).astype(np.float32)
    NO = NLB + 2
    vt = np.full((NO, 32), -1e30, np.float32)
    ow = np.zeros((NO, 32), np.float32)
    pvb = 0.0 if second_half else -1e30
    for o in range(NO):
        n_own = NPB - 2 + o
        for n in range(32):
            if n < n_own:
                vt[o, n] = pvb if n < NPB else 0.0
        if 0 <= n_own < 32:
            ow[o, n_own] = 1.0
    c["vtab"] = np.ascontiguousarray(np.broadcast_to(vt[None], (128, NO, 32)))
    c["own01"] = np.ascontiguousarray(np.broadcast_to(ow[None], (128, NO, 32)))
    c["pv"] = np.full((128, 1), 1.0 if second_half else 0.0, np.float32)
    return c


def _params(inp):
    f = lambda a: np.ascontiguousarray(np.asarray(a, dtype=np.float32))
    pr = {}
    pr["w_in"] = f(inp["w_in"][0])
    pr["w_ssd"] = f(inp["w_ssd_branch"][0])
    pr["w_attn"] = f(inp["w_attn_branch"][0])
    pr["w_out"] = f(inp["w_out"][0])
    pr["w_up"] = f(inp["w_ffn_up"][0])
    pr["w_down"] = f(inp["w_ffn_down"][0])
    pr["g_pre_mix"] = f(np.asarray(inp["pre_mix_norm"][0]).reshape(8, 128).T)
    pr["g_pre_ffn"] = f(np.asarray(inp["pre_ffn_norm"][0]).reshape(8, 128).T)
    pr["g_post_mix"] = f(np.asarray(inp["post_mix_norm"][0]).reshape(1, D))
    pr["g_post_ffn"] = f(np.asarray(inp["post_ffn_norm"][0]).reshape(1, D))
    pr["cw"] = f(np.asarray(inp["ssd_conv_w"][0]).T.reshape(24, 128, 4).transpose(1, 0, 2))
    pr["cb"] = f(np.asarray(inp["ssd_conv_b"][0]).reshape(24, 128).T)
    pr["dtb"] = f(np.tile(np.asarray(inp["ssd_dt_bias"][0]), 2).reshape(64, 1))
    pr["alog"] = f(np.tile(np.asarray(inp["ssd_a_log"][0]), 2).reshape(64, 1))
    pr["dsk"] = f(np.repeat(np.asarray(inp["ssd_d_skip"][0]), 64).reshape(16, 128).T)
    pr["gon"] = f(np.asarray(inp["ssd_out_norm"][0]).reshape(16, 128).T)
    pr["fcw"] = f(np.asarray(inp["ffn_conv_w"][0]).T.reshape(32, 128, 3).transpose(1, 0, 2))
    pr["fcb"] = f(np.asarray(inp["ffn_conv_b"][0]).reshape(32, 128).T)
    return pr


def run_cores(inp, NP, NM, cores, dbg=(), dbg_tile=None):
    nc, counts, dbg_out = build_nc(NP, NM, dbg=dbg, dbg_tile=dbg_tile)
    pr = _params(inp)
    x = np.asarray(inp["x"], dtype=np.float32)
    W = (NP + NM) * TT
    in_maps = []
    for (b, hf) in cores:
        m = dict(pr)
        m.update(_consts(NP, NM, hf == 1))
        xw = np.zeros((W, D), np.float32)
        if hf == 0:
            xw[NP * TT:] = x[b, :NM * TT]
        else:
            xw[:] = x[b, :W]
        m["xw"] = xw
        in_maps.append(m)
    res = run_bass_kernel_spmd(nc, in_maps, core_ids=list(range(len(cores))))
    return res, counts


def kernel(**inputs):
    NP, NM = 8, 8
    cores = [(b, hf) for b in range(4) for hf in range(2)]
    res, _ = run_cores(inputs, NP, NM, cores)
    x = np.asarray(inputs["x"])
    out = np.empty(x.shape, np.float32)
    for i, (b, hf) in enumerate(cores):
        out[b, hf * NM * TT:(hf + 1) * NM * TT] = res.results[i]["out"]
    return out
```

```python
import numpy as np
import ml_dtypes
from contextlib import ExitStack
import concourse.bass as bass
import concourse.mybir as mybir
from concourse.bass_utils import run_bass_kernel_spmd

F32 = mybir.dt.float32
BF16 = mybir.dt.bfloat16
AF = mybir.ActivationFunctionType
ALU = mybir.AluOpType
AX = mybir.AxisListType
PE, ACT, DVE, POOL, SP = "tensor", "scalar", "vector", "gpsimd", "sync"
ENGS = (PE, ACT, DVE, POOL, SP)
NPBF = ml_dtypes.bfloat16

D = 1024
TT = 512
NCOL_IN = 10272
OFF_Z, OFF_XBC, OFF_DT, OFF_Q, OFF_K, OFF_V, OFF_GS, OFF_GA = 0, 2048, 5120, 5152, 6176, 7200, 8224, 9248
BIG = 30000.0
EPS = 1e-6
KU = 1024
NSU = KU // 128
SLOPES = [2.0 ** (-8.0 * (h + 1) / 16) for h in range(16)]
ALIBI_CUT = 80.0
SEMI_SHORT = True


class Buf:
    __slots__ = ("ap", "w", "r", "name", "root", "psum")

    def __init__(self, ap, name="", root=None, psum=False):
        self.ap = ap
        self.w = None
        self.r = []
        self.name = name
        self.root = root.root if root is not None else self
        self.psum = psum or (root is not None and root.root.psum)


class Op:
    __slots__ = ("eng", "fn", "deps", "sig", "ms", "dma", "sem", "target")

    def __init__(self, eng, fn, deps, dma):
        self.eng, self.fn, self.deps, self.dma = eng, fn, deps, dma
        self.sig, self.ms, self.sem, self.target = False, 0, None, 0


class Prog:
    def __init__(self, nc, n_dma_sems=14):
        self.nc = nc
        self.ops = {e: [] for e in ENGS}
        self.n_dma_sems = n_dma_sems
        self.dma_rr = {e: 0 for e in ENGS}
        self.dma_cnt = {}
        self.dma_last = {}

    def emit(self, eng, fn, reads=(), writes=(), dma=False, same_ok=False):
        deps, seen = [], set()
        reads_, writes_ = [], []
        for b in reads:
            (writes_ if b.root.psum else reads_).append(b.root)
        for b in writes:
            writes_.append(b.root)
        reads, writes = reads_, writes_
        touched = [b for b in reads + writes if not b.psum]
        cand = []
        for b in reads:
            if b.w is not None:
                cand.append(b.w)
        for b in writes:
            if b.w is not None:
                cand.append(b.w)
            cand.extend(b.r)
        for d in cand:
            if id(d) in seen:
                continue
            seen.add(id(d))
            if d.eng == eng and not d.dma:
                if eng == PE or eng == SP or same_ok:
                    continue
            deps.append(d)
        op = Op(eng, fn, deps, dma)
        if dma:
            slot = self.dma_rr[eng] % self.n_dma_sems
            self.dma_rr[eng] += 1
            key = (eng, slot)
            prev = self.dma_last.get(key)
            if prev is not None:
                op.deps.append(prev)
            self.dma_cnt[key] = self.dma_cnt.get(key, 0) + 1
            op.sem, op.target = key, 16 * self.dma_cnt[key]
            self.dma_last[key] = op
        for d in op.deps:
            d.sig = True
        for b in reads:
            b.r.append(op)
        for b in writes:
            b.w = op
            b.r = []
        self.ops[eng].append(op)
        return op

    def finalize(self, final_ops):
        nc = self.nc
        for o in final_ops:
            o.sig = True
        for e in ENGS:
            c = 0
            for op in self.ops[e]:
                if not op.dma and op.sig:
                    c += 1
                    op.ms = c
        with ExitStack() as st:
            esem = {e: st.enter_context(nc.semaphore("s_" + e)) for e in ENGS}
            dsem = {k: st.enter_context(nc.semaphore("d_%s_%d" % k)) for k in self.dma_cnt}
            block = st.enter_context(nc.Block())

            def run(e, h, last=False):
                waited = {}
                for op in self.ops[e]:
                    for d in op.deps:
                        if d.dma:
                            s, v, k = dsem[d.sem], d.target, ("d",) + d.sem
                        else:
                            s, v, k = esem[d.eng], d.ms, ("e", d.eng)
                        if waited.get(k, 0) >= v:
                            continue
                        waited[k] = v
                        h.wait_ge(s, v)
                    ins = op.fn(h)
                    if op.dma:
                        ins.then_inc(dsem[op.sem], 16)
                    elif op.sig:
                        ins.then_inc(esem[e], 1)
                if last:
                    for d in final_ops:
                        if d.dma:
                            h.wait_ge(dsem[d.sem], d.target)
                        else:
                            h.wait_ge(esem[d.eng], d.ms)

            block.tensor(lambda h: run(PE, h))
            block.scalar(lambda h: run(ACT, h))
            block.vector(lambda h: run(DVE, h))
            block.gpsimd(lambda h: run(POOL, h))
            block.sync(lambda h: run(SP, h, last=True))
        return {e: len(self.ops[e]) for e in ENGS}


def build_nc(NP, NM, dbg=(), dbg_tile=None):
    W = (NP + NM) * TT
    NPB, NLB = 2 * NP, 2 * NM
    NOFFMAX = (NP + NM - 1) * 4
    NOFF = NOFFMAX + 4
    NUNITS = (W + KU - 1) // KU
    nc = bass.Bass("TRN2", target_bir_lowering=False)
    P = Prog(nc)
    din = lambda n, s, d: nc.dram_tensor(n, list(s), d, kind="ExternalInput").ap()
    dsc = lambda n, s, d: nc.dram_tensor(n, list(s), d).ap()

    xw = din("xw", [W, D], F32)
    w_in = din("w_in", [D, NCOL_IN], F32)
    w_ssd = din("w_ssd", [2048, D], F32)
    w_attn = din("w_attn", [D, D], F32)
    w_out = din("w_out", [D, D], F32)
    w_up = din("w_up", [D, 8192], F32)
    w_down = din("w_down", [4096, D], F32)
    g_pre_mix = din("g_pre_mix", [128, 8], F32)
    g_pre_ffn = din("g_pre_ffn", [128, 8], F32)
    g_post_mix = din("g_post_mix", [1, D], F32)
    g_post_ffn = din("g_post_ffn", [1, D], F32)
    cw_d = din("cw", [128, 24, 4], F32)
    cb_d = din("cb", [128, 24], F32)
    dtb_d = din("dtb", [64, 1], F32)
    alog_d = din("alog", [64, 1], F32)
    dsk_d = din("dsk", [128, 16], F32)
    gon_d = din("gon", [128, 16], F32)
    fcw_d = din("fcw", [128, 32, 3], F32)
    fcb_d = din("fcb", [128, 32], F32)
    identb_d = din("identb", [128, 128], BF16)
    identf_d = din("identf", [64, 64], F32)
    selcol_d = din("selcol", [64, 32], F32)
    triu_d = din("triu", [128, 128], F32)
    cmask_d = din("cmask", [128, 4, 512], BF16)
    kaug_d = din("kaug", [33, NUNITS * KU], BF16)
    qsh_d = din("qsh", [128, 16, 4], F32)
    abias_d = din("abias", [128, 16, NOFF], F32)
    vtab_d = din("vtab", [128, NLB + 2, 32], F32)
    own_d = din("own01", [128, NLB + 2, 32], F32)
    pv_d = din("pv", [128, 1], F32)
    out_d = nc.dram_tensor("out", [NM * TT, D], F32, kind="ExternalOutput").ap()
    dbg_out = {}

    wb_in = dsc("wb_in", [D, NCOL_IN], BF16)
    wb_ssd = dsc("wb_ssd", [2048, D], BF16)
    wb_attn = dsc("wb_attn", [D, D], BF16)
    wb_out = dsc("wb_out", [D, D], BF16)
    wb_up = dsc("wb_up", [D, 8192], BF16)
    wb_down = dsc("wb_down", [4096, D], BF16)
    Ks = dsc("Ks", [16, 64, NUNITS * KU], BF16)
    Vs = dsc("Vs", [16, 128, NUNITS * NSU, 64], BF16)

    st = ExitStack()
    with st:
        def sb(name, shape, dt):
            return Buf(st.enter_context(nc.sbuf_tensor("sb_" + name, list(shape), dt)), name)

        def ps(name, shape, dt=F32):
            return Buf(st.enter_context(nc.psum_tensor("ps_" + name, list(shape), dt)), name, psum=True)

        def sub(b, ap, name=""):
            return Buf(ap, name)

        DIN = Buf(None, "dram_in")
        wbB = {}

        def E(eng, fn, reads=(), writes=(), **kw):
            return P.emit(eng, fn, reads=reads, writes=writes, **kw)

        def dma(eng, out_ap, in_ap, reads, writes):
            return P.emit(eng, lambda e: e.dma_start(out=out_ap, in_=in_ap), reads=reads, writes=writes, dma=True)

        identb = sb("identb", [128, 128], BF16)
        identf = sb("identf", [64, 64], F32)
        selcol = sb("selcol", [64, 32], F32)
        triu = sb("triu", [128, 128], F32)
        onesb = sb("onesb", [128, 128], BF16)
        onesf = sb("onesf", [128, 128], F32)
        cmask = sb("cmask", [128, 4, 512], BF16)
        qsh = sb("qsh", [128, 16, 4], F32)
        abias = sb("abias", [128, 16, NOFF], F32)
        vtab = sb("vtab", [128, NLB + 2, 32], F32)
        own01 = sb("own01", [128, NLB + 2, 32], F32)
        pv = sb("pv", [128, 1], F32)
        gpm = sb("gpm", [128, 8], F32)
        gpf = sb("gpf", [128, 8], F32)
        gqm = sb("gqm", [128, D], F32)
        gqf = sb("gqf", [128, D], F32)
        cw = sb("cw", [128, 24, 4], F32)
        cb = sb("cb", [128, 24], F32)
        dtb = sb("dtb", [64, 1], F32)
        aneg = sb("aneg", [64, 1], F32)
        dsk = sb("dsk", [128, 16], F32)
        gon = sb("gon", [128, 16], F32)
        fcw = sb("fcw", [128, 32, 3], F32)
        fcb = sb("fcb", [128, 32], F32)
        for t, s in ((identb, identb_d), (identf, identf_d), (selcol, selcol_d), (triu, triu_d), (cmask, cmask_d), (qsh, qsh_d),
                     (abias, abias_d), (vtab, vtab_d), (own01, own_d), (pv, pv_d), (gpm, g_pre_mix), (gpf, g_pre_ffn),
                     (cw, cw_d), (cb, cb_d), (dtb, dtb_d), (aneg, alog_d), (dsk, dsk_d), (gon, gon_d), (fcw, fcw_d), (fcb, fcb_d)):
            dma(SP, t.ap[:], s, [DIN], [t])
        dma(SP, gqm.ap[:], g_post_mix.partition_broadcast(128), [DIN], [gqm])
        dma(SP, gqf.ap[:], g_post_ffn.partition_broadcast(128), [DIN], [gqf])
        E(DVE, lambda e: e.memset(onesb.ap[:], 1.0), writes=[onesb])
        E(DVE, lambda e: e.memset(onesf.ap[:], 1.0), writes=[onesf])
        E(ACT, lambda e: e.activation(out=aneg.ap[:], in_=aneg.ap[:], func=AF.Exp), reads=[aneg], writes=[aneg])
        E(DVE, lambda e: e.tensor_scalar(out=aneg.ap[:], in0=aneg.ap[:], scalar1=-1.0, scalar2=None, op0=ALU.mult), reads=[aneg], writes=[aneg])

        xt = sb("xt", [128, 4, D], F32)
        xt_s = [Buf(xt.ap[:, s_, :]) for s_ in range(4)]
        hT = sb("hT", [128, 8, TT], BF16)
        junk = sb("junk", [128, D], BF16)
        ssq = sb("ssq", [128, 4], F32)
        rstd = sb("rstd", [128, 4], F32)
        Gall = sb("Gall", [128, 32, TT], BF16)
        G = [Buf(Gall.ap[:, i, :], "G%d" % i) for i in range(32)]
        xbc, qT, act = G[0:24], G[24:32], G
        Hall = sb("Hall", [128, 16, TT], BF16)
        yT = [Buf(Hall.ap[:, i, :], "yT%d" % i) for i in range(16)]
        attall = sb("attall", [64, 16, TT], BF16)
        attT = [Buf(attall.ap[:, i, :], "att%d" % i) for i in range(16)]
        NWB = 3
        wbufs = [sb("wbuf%d" % i, [128, 4096], BF16) for i in range(NWB)]
        fT = [sb("fT%d" % i, [128, TT], F32) for i in range(4)]
        stage = [sb("stage%d" % i, [128, TT + 3], BF16) for i in range(3)]
        stage_h = [Buf(stage[i].ap[:, 0:3]) for i in range(3)]
        stage_d = [Buf(stage[i].ap[:, 3:TT + 3]) for i in range(3)]
        halo_all = sb("halo", [128, 24, 3], BF16)
        halo = [Buf(halo_all.ap[:, j, :]) for j in range(24)]
        fhalo_all = sb("fhalo", [128, 32, 2], BF16)
        fhalo = [Buf(fhalo_all.ap[:, j, :]) for j in range(32)]
        dtacs = sb("dtacs", [64, TT], F32)
        state = sb("state", [128, 2048], F32)
        stbf = sb("stbf", [128, 2048], BF16)
        state_h = [Buf(state.ap[:, h_ * 64:(h_ + 1) * 64]) for h_ in range(32)]
        stbf_g = [Buf(stbf.ap[:, g_ * 512:(g_ + 1) * 512]) for g_ in range(4)]
        kmT = sb("kmT", [128, 8, 32], BF16)
        ksum = sb("ksum", [128, 2], F32)
        NKB = 3
        Kt = [sb("Kt%d" % i, [128, KU], BF16) for i in range(NKB)]
        Kt_k = [Buf(Kt[i].ap[0:64, :]) for i in range(NKB)]
        Kt_a = [Buf(Kt[i].ap[64:97, :]) for i in range(NKB)]
        Vt = [sb("Vt%d" % i, [128, NSU, 65], BF16) for i in range(NKB)]
        rbufs = [sb("rbuf%d" % i, [128, D], F32) for i in range(2)]
        tm = sb("tm", [128, 64], F32)
        elast = sb("elast", [128, 32], F32)
        wend = sb("wend", [128, 32], F32)
        xdt = [sb("xdt%d" % i, [128, 512], BF16) for i in range(2)]
        xdtw = [sb("xdtw%d" % i, [128, 512], BF16) for i in range(2)]
        Btm = [sb("Btm%d" % i, [128, 128], BF16) for i in range(2)]
        cbm = sb("cbm", [128, 4, 128], F32)
        t2b = [sb("t2b%d" % i, [128, 512], BF16) for i in range(2)]
        e2b = [sb("e2b%d" % i, [128, 512], BF16) for i in range(2)]
        Mhb = [sb("Mhb%d" % i, [128, 512], BF16) for i in range(3)]
        Cpb = [sb("Cpb%d" % i, [128, 512], BF16) for i in range(3)]
        Qaug = [sb("Qaug%d" % i, [128, TT], BF16) for i in range(2)]
        gstage = sb("gstage", [128, 4, 97], BF16)
        gm = sb("gm", [128, 4, 32], F32)
        m8 = sb("m8", [128, 4, 8], F32)
        selb = sb("selb", [128, 4, 32], F32)
        vbt = sb("vbt", [128, 4, 32], F32)
        v01t = sb("v01t", [128, 4, 32], F32)
        ownt = sb("ownt", [128, 4, 32], F32)
        pT = [sb("pT%d" % i, [128, TT], BF16) for i in range(2)]
        pm = [ps("pm%d" % i, [128, 512]) for i in range(4)]
        pa0 = ps("pa0", [128, 512])
        pa1 = ps("pa1", [128, 512])
        pt1 = ps("pt1", [128, 512])
        pt0 = ps("pt0", [128, 1024], BF16)
        pt0a, pt0b = Buf(pt0.ap[:, 0:512], root=pt0), Buf(pt0.ap[:, 512:1024], root=pt0)
        pt1bf = Buf(pt1.ap[:, :].bitcast(BF16)[:, 0:512], root=pt1)
        pa0a, pa0b, pa0c = Buf(pa0.ap[:, 0:64], root=pa0), Buf(pa0.ap[:, 64:96], root=pa0), Buf(pa0.ap[:, 128:256], root=pa0)
        bcq = [Buf(pm[i].ap[:, 0:128], root=pm[i]) for i in range(2)]

        E(DVE, lambda e: e.memset(halo_all.ap[:], 0.0), writes=halo)
        E(POOL, lambda e: e.memset(Hall.ap[:], 0.0), writes=yT)
        E(POOL, lambda e: e.memset(attall.ap[:], 0.0), writes=attT)
        E(DVE, lambda e: e.memset(fhalo_all.ap[:], 0.0), writes=fhalo)
        E(DVE, lambda e: e.memset(state.ap[:], 0.0), writes=state_h)
        E(DVE, lambda e: e.memset(stbf.ap[:], 0.0), writes=stbf_g)
        E(DVE, lambda e: e.memset(gstage.ap[:], 0.0), writes=[gstage])
        E(DVE, lambda e: e.memset(kmT.ap[:], 0.0), writes=[kmT])
        for i in range(NKB):
            E(DVE, lambda e, i=i: e.memset(Vt[i].ap[:], 1.0), writes=[Vt[i]])
            E(DVE, lambda e, i=i: e.memset(Kt[i].ap[:], 0.0), writes=[Kt_k[i], Kt_a[i]])

        rr = {"pm": 0, "wb": 0, "ft": 0, "ku": 0, "pt": 0, "bq": 0, "stg": 0, "f8": 0}
        hs_ap = [rbufs[s_ // 2].ap[:, :].bitcast(BF16)[:, (s_ % 2) * D:(s_ % 2 + 1) * D] for s_ in range(4)]
        hs_b = [rbufs[s_ // 2] for s_ in range(4)]
        KsB, VsB = {}, {}
        dbg_final = []

        def dump(name, buf, shape, dt, tt_, bufs=None):
            if name not in dbg or tt_ != dbg_tile:
                return
            d_ = nc.dram_tensor("dbg_" + name, list(shape), dt, kind="ExternalOutput").ap()
            dbg_out[name] = d_
            dbg_final.append(dma(SP, d_, buf.ap[:], bufs if bufs is not None else [buf], [Buf(None)]))

        def nxt(key, lst):
            i = rr[key]
            rr[key] = i + 1
            return lst[i % len(lst)]

        cast_jobs = []

        def add_cast(name, wsrc, wdst, r0, r1, c0, c1):
            b = Buf(None, "wb_%s_%d_%d" % (name, r0, c0))
            wbB.setdefault(name, []).append((r0, r1, c0, c1, b))
            cast_jobs.append((b, wdst[r0:r1, c0:c1], wsrc[r0:r1, c0:c1]))

        def cast_cols(name, wsrc, wdst, nrows, c0, c1, step=512):
            c = c0
            while c < c1:
                add_cast(name, wsrc, wdst, 0, nrows, c, min(c + step, c1))
                c += step

        cast_cols("in", w_in, wb_in, D, OFF_XBC, OFF_XBC + 2560)
        cast_cols("in", w_in, wb_in, D, OFF_DT, OFF_DT + 32)
        cast_cols("in", w_in, wb_in, D, OFF_K, OFF_K + 2048)
        n_cast_first = len(cast_jobs)
        cast_cols("in", w_in, wb_in, D, OFF_XBC + 2560, OFF_XBC + 3072)
        cast_cols("in", w_in, wb_in, D, OFF_Q, OFF_Q + 1024)
        cast_cols("in", w_in, wb_in, D, OFF_Z, OFF_Z + 2048)
        cast_cols("in", w_in, wb_in, D, OFF_GS, OFF_GS + 2048)
        for r in range(0, 2048, 512):
            add_cast("ssd", w_ssd, wb_ssd, r, r + 512, 0, D)
        for r in range(0, D, 512):
            add_cast("attn", w_attn, wb_attn, r, r + 512, 0, D)
        for r in range(0, D, 512):
            add_cast("out", w_out, wb_out, r, r + 512, 0, D)
        cast_cols("up", w_up, wb_up, D, 0, 8192)
        for r in range(0, 4096, 512):
            add_cast("down", w_down, wb_down, r, r + 512, 0, D)
        cast_state = {"i": 0}

        def issue_casts(n):
            while n > 0 and cast_state["i"] < len(cast_jobs):
                b, o, i_ = cast_jobs[cast_state["i"]]
                cast_state["i"] += 1
                dma(POOL, o, i_, [DIN], [b])
                n -= 1

        def wdeps(name, r0, r1, c0, c1):
            res = []
            for (a0, a1, b0, b1, b) in wbB[name]:
                if a0 < r1 and r0 < a1 and b0 < c1 and c0 < b1:
                    res.append(b)
            assert res, (name, r0, r1, c0, c1)
            return res

        def load_panel(name, wdram, KC, c0, ncols, dup=False):
            wbf = nxt("wb", wbufs)
            PW = 4096 // KC
            view = wbf.ap[:, :].rearrange("p (c n) -> p c n", c=KC)
            src = wdram[:, c0:c0 + ncols].rearrange("(c p) n -> p c n", p=128)
            deps = wdeps(name, 0, KC * 128, c0, c0 + ncols)
            dma(SP, view[:, :, 0:ncols], src, deps, [wbf])
            if dup:
                dma(SP, view[:, :, ncols:2 * ncols], src, deps, [wbf])
            return wbf, view

        def proj_fm(name, wdram, KC, c0, ntiles, srcT, consume, ts=0):
            ptiles = (4096 // KC) // 128
            pend = []
            for p0 in range(0, ntiles, ptiles):
                npan = min(ptiles, ntiles - p0)
                wbf, view = load_panel(name, wdram, KC, c0 + p0 * 128, npan * 128)
                for j in range(npan):
                    pmb = nxt("pm", pm)
                    for c in range(KC):
                        E(PE, lambda e, c=c, j=j, pmb=pmb, view=view: e.matmul(pmb.ap[:, ts:TT], lhsT=view[:, c, j * 128:(j + 1) * 128], rhs=srcT[c][:, ts:TT],
                                                                                  start=(c == 0), stop=(c == KC - 1)),
                          reads=[wbf, (srcT.bufs[c] if len(srcT.bufs) > 1 else srcT.bufs[0])], writes=[pmb])
                    stages = consume(p0 + j, pmb)
                    if stages:
                        stages[0]()
                        for rest in reversed(pend):
                            if rest:
                                rest.pop(0)()
                        pend.append(list(stages[1:]))
            while any(pend):
                for rest in reversed(pend):
                    if rest:
                        rest.pop(0)()

        def srcT_bufs(srcT):
            return srcT.bufs

        class Src:
            def __init__(self, aps, bufs):
                self.aps, self.bufs = aps, bufs

            def __getitem__(self, c):
                return self.aps[c]

        def rms_rstd(src_ap_fn, src_bufs, n):
            for s in range(4):
                E(ACT, lambda e, s=s: e.activation(out=junk.ap[:, 0:n], in_=src_ap_fn(s), func=AF.Square, accum_out=ssq.ap[:, s:s + 1]),
                  reads=([src_bufs[s]] if len(src_bufs) == 4 else src_bufs), writes=[junk, ssq])
            E(ACT, lambda e: e.activation(out=rstd.ap[:], in_=ssq.ap[:], func=AF.Ln, scale=1.0 / n, bias=EPS), reads=[ssq], writes=[rstd])
            E(ACT, lambda e: e.activation(out=rstd.ap[:], in_=rstd.ap[:], func=AF.Exp, scale=-0.5), reads=[rstd], writes=[rstd])

        def norm_transpose(gain_fm):
            rms_rstd(lambda s: xt.ap[:, s, :], xt_s, D)
            for s in range(4):
                E(DVE, lambda e, s=s: e.tensor_scalar(out=hs_ap[s], in0=xt.ap[:, s, :], scalar1=rstd.ap[:, s:s + 1], scalar2=None, op0=ALU.mult),
                  reads=[xt_s[s], rstd], writes=[hs_b[s]], same_ok=True)
            for c in range(8):
                ptb = pt0a if c % 2 == 0 else pt1bf
                for s in range(4):
                    E(PE, lambda e, c=c, s=s, ptb=ptb: e.transpose(out=ptb.ap[:, s * 128:(s + 1) * 128], in_=hs_ap[s][:, c * 128:(c + 1) * 128], identity=identb.ap[:]),
                      reads=[hs_b[s], identb], writes=[ptb])
                eng = DVE if c % 2 == 0 else ACT
                if eng == DVE:
                    E(DVE, lambda e, c=c, ptb=ptb: e.tensor_scalar(out=hT.ap[:, c, :], in0=ptb.ap[:, :], scalar1=gain_fm.ap[:, c:c + 1], scalar2=None, op0=ALU.mult),
                      reads=[ptb, gain_fm], writes=[hT], same_ok=True)
                else:
                    E(ACT, lambda e, c=c, ptb=ptb: e.mul(out=hT.ap[:, c, :], in_=ptb.ap[:, :], mul=gain_fm.ap[:, c:c + 1]),
                      reads=[ptb, gain_fm], writes=[hT], same_ok=True)

        hT_src = Src([hT.ap[:, c, :] for c in range(8)], [hT])

        final_ops = []
        for tt in range(NP + NM):
            is_main = tt >= NP
            mixfull = tt >= NP - 1
            TS = (384 if SEMI_SHORT else 0) if (NP > 0 and tt == NP - 1) else 0
            SR = range(TS // 128, 4)
            tm_i = tt - NP
            tok0 = tt * TT
            issue_casts(10 ** 9 if tt >= NP - 1 else (n_cast_first if tt == 0 else 8))
            for s_ in range(4):
                dma(SP, xt.ap[:, s_, :], xw[tok0 + s_ * 128:tok0 + (s_ + 1) * 128, :], [DIN], [xt_s[s_]])
            norm_transpose(gpm)

            def conv_consume(j, pmb, base=0):
                jj = base + j
                si = rr["stg"] % 3
                rr["stg"] += 1
                stg, sh, sd = stage[si], stage_h[si], stage_d[si]
                tmp = nxt("ft", fT)

                def s0():
                    E(POOL, lambda e: e.tensor_copy(out=stg.ap[:, 0:3], in_=halo[jj].ap), reads=[halo[jj]], writes=[sh])
                    E(ACT, lambda e: e.activation(out=stg.ap[:, 3:TT + 3], in_=pmb.ap[:, :], func=AF.Copy), reads=[pmb], writes=[sd])
                    E(POOL, lambda e: e.tensor_copy(out=halo[jj].ap, in_=stg.ap[:, TT:TT + 3]), reads=[sd], writes=[halo[jj]])
                    E(ACT, lambda e: e.activation(out=tmp.ap[:, :], in_=stg.ap[:, 0:TT], func=AF.Identity, scale=cw.ap[:, jj, 0:1], bias=cb.ap[:, jj:jj + 1]),
                      reads=[sh, sd, cw, cb], writes=[tmp])

                def s1():
                    for k in range(1, 4):
                        E(DVE, lambda e, k=k: e.scalar_tensor_tensor(out=tmp.ap[:, :], in0=stg.ap[:, k:k + TT], scalar=cw.ap[:, jj, k:k + 1], in1=tmp.ap[:, :], op0=ALU.mult, op1=ALU.add),
                          reads=[sh, sd, cw, tmp], writes=[tmp])

                def s2():
                    E(ACT, lambda e: e.activation(out=xbc[jj].ap, in_=tmp.ap[:, :], func=AF.Silu), reads=[tmp], writes=[xbc[jj]])

                return [s0, s1, s2]

            nxbc = 24 if mixfull else 20
            proj_fm("in", wb_in, 8, OFF_XBC, nxbc, hT_src, conv_consume)

            wbf, view = load_panel("in", wb_in, 8, OFF_DT, 32, dup=True)
            pmb = nxt("pm", pm)
            dttmp = nxt("ft", fT)
            for c in range(8):
                E(PE, lambda e, c=c, pmb=pmb, view=view: e.matmul(pmb.ap[0:64, :], lhsT=view[:, c, 0:64], rhs=hT.ap[:, c, :], start=(c == 0), stop=(c == 7)),
                  reads=[wbf, hT], writes=[pmb])
            E(ACT, lambda e, pmb=pmb, dttmp=dttmp: e.activation(out=dttmp.ap[0:64, :], in_=pmb.ap[0:64, :], func=AF.Exp, bias=dtb.ap[:, 0:1]), reads=[pmb, dtb], writes=[dttmp])
            E(ACT, lambda e, dttmp=dttmp: e.activation(out=dtacs.ap[:, :], in_=dttmp.ap[0:64, :], func=AF.Ln, bias=1.0), reads=[dttmp], writes=[dtacs])
            E(DVE, lambda e, dttmp=dttmp: e.tensor_scalar(out=dttmp.ap[32:64, :], in0=dtacs.ap[32:64, :], scalar1=aneg.ap[32:64, 0:1], scalar2=None, op0=ALU.mult),
              reads=[dtacs, aneg], writes=[dttmp])
            for ci in range(4):
                E(DVE, lambda e, ci=ci, dttmp=dttmp: e.tensor_tensor_scan(out=dtacs.ap[32:64, ci * 128:(ci + 1) * 128], data0=onesf.ap[32:64, :],
                                                              data1=dttmp.ap[32:64, ci * 128:(ci + 1) * 128], initial=0.0, op0=ALU.mult, op1=ALU.add),
                  reads=[dttmp, onesf], writes=[dtacs], same_ok=(ci > 0))
            dump("dtacs", dtacs, [64, TT], F32, tt)

            KsB[tt] = Buf(None, "Ks%d" % tt)
            VsB[tt] = Buf(None, "Vs%d" % tt)

            def k_consume(j, pmb):
                kb = yT[8 + j % 2]
                E(ACT, lambda e: e.activation(out=kb.ap, in_=pmb.ap[:, :], func=AF.Copy), reads=[pmb], writes=[kb])
                E(DVE, lambda e: e.tensor_reduce(out=ksum.ap[:, :], in_=kb.ap.rearrange("p (b t) -> p b t", b=2), axis=AX.X, op=ALU.add),
                  reads=[kb], writes=[ksum])
                km_ap = kmT.ap[:, j, 2 * tt:2 * tt + 2]
                E(DVE, lambda e: e.tensor_scalar(out=km_ap, in0=ksum.ap[:, :], scalar1=1.0 / 256, scalar2=None, op0=ALU.mult),
                  reads=[ksum], writes=[kmT])
                dma(SP, Ks[2 * j:2 * j + 2, :, tok0:tok0 + TT].rearrange("h r t -> (h r) t"), kb.ap, [kb], [KsB[tt]])

            proj_fm("in", wb_in, 8, OFF_K, 8, hT_src, k_consume)

            wv = [load_panel("in", wb_in, 8, OFF_V + hf * 512, 512) for hf in range(2)]
            for s in range(4):
                vi = 10 + 2 * (s % 2)
                vb_bufs = [yT[vi], yT[vi + 1]]
                vb_ap = Hall.ap[:, vi:vi + 2, :].rearrange("p a t -> p (a t)")
                for hf in range(2):
                    wbf, view = wv[hf]
                    pmb = nxt("pm", pm)
                    for c in range(8):
                        E(PE, lambda e, c=c, s=s, pmb=pmb, view=view: e.matmul(pmb.ap[:, :], lhsT=hT.ap[:, c, s * 128:(s + 1) * 128], rhs=view[:, c, 0:512],
                                                                                  start=(c == 0), stop=(c == 7)),
                          reads=[wbf, hT], writes=[pmb])
                    if hf == 0:
                        E(DVE, lambda e, pmb=pmb, vb_ap=vb_ap: e.tensor_copy(out=vb_ap[:, 0:512], in_=pmb.ap[:, :]), reads=[pmb], writes=[vb_bufs[0]])
                    else:
                        E(ACT, lambda e, pmb=pmb, vb_ap=vb_ap: e.activation(out=vb_ap[:, 512:1024], in_=pmb.ap[:, :], func=AF.Copy), reads=[pmb], writes=[vb_bufs[1]])
                dma(SP, Vs[:, :, tt * 4 + s, :].rearrange("h p d -> p h d"), vb_ap.rearrange("p (h d) -> p h d", h=16), vb_bufs, [VsB[tt]])

            def ssd_chunk(ci):
                c0, c1 = ci * 128, (ci + 1) * 128
                doy = mixfull and c0 >= TS

                def C_pro():
                    E(PE, lambda e: e.matmul(pa0a.ap[:, 0:64], lhsT=dtacs.ap[0:64, c0:c1], rhs=identf.ap[:, :], start=True, stop=True),
                      reads=[dtacs, identf], writes=[pa0a])
                    E(DVE, lambda e: e.tensor_copy(out=tm.ap[:, :], in_=pa0a.ap[:, 0:64]), reads=[pa0a], writes=[tm])
                    E(PE, lambda e: e.matmul(pa0b.ap[:, 0:32], lhsT=dtacs.ap[0:64, c1 - 1:c1].to_broadcast([64, 128]), rhs=selcol.ap[:, :], start=True, stop=True),
                      reads=[dtacs, selcol], writes=[pa0b])
                    E(ACT, lambda e: e.activation(out=elast.ap[:, :], in_=pa0b.ap[:, 0:32], func=AF.Exp), reads=[pa0b], writes=[elast])
                    E(DVE, lambda e: e.tensor_tensor(out=wend.ap[:, :], in0=pa0b.ap[:, 0:32], in1=tm.ap[:, 32:64], op=ALU.subtract), reads=[pa0b, tm], writes=[wend])
                    E(ACT, lambda e: e.activation(out=wend.ap[:, :], in_=wend.ap[:, :], func=AF.Exp), reads=[wend], writes=[wend])
                    if doy:
                        for g in range(4):
                            E(PE, lambda e, g=g: e.matmul(pa1.ap[:, g * 128:(g + 1) * 128], lhsT=xbc[16 + g].ap[:, c0:c1], rhs=xbc[20 + g].ap[:, c0:c1], start=True, stop=True),
                              reads=[xbc[16 + g], xbc[20 + g]], writes=[pa1])
                        E(DVE, lambda e: e.tensor_tensor(out=cbm.ap[:, :, :], in0=pa1.ap[:, :].rearrange("p (g l) -> p g l", g=4),
                                                         in1=triu.ap[:, :].unsqueeze(1).to_broadcast([128, 4, 128]), op=ALU.mult),
                          reads=[pa1, triu], writes=[cbm])

                def G_pro1(g):
                    for k in range(4):
                        E(PE, lambda e, k=k: e.transpose(out=pt0a.ap[:, k * 128:(k + 1) * 128], in_=xbc[4 * g + k].ap[:, c0:c1], identity=identb.ap[:]),
                          reads=[xbc[4 * g + k], identb], writes=[pt0a])
                    E(PE, lambda e: e.transpose(out=pt0b.ap[:, 0:128], in_=xbc[16 + g].ap[:, c0:c1], identity=identb.ap[:]),
                      reads=[xbc[16 + g], identb], writes=[pt0b])
                    xd, xdw, bt = xdt[g % 2], xdtw[g % 2], Btm[g % 2]
                    E(DVE, lambda e: e.tensor_tensor(out=xd.ap[:, :].rearrange("p (h d) -> p h d", h=8), in0=pt0a.ap[:, :].rearrange("p (h d) -> p h d", h=8),
                                                     in1=tm.ap[:, 8 * g:8 * g + 8].unsqueeze(2).to_broadcast([128, 8, 64]), op=ALU.mult),
                      reads=[pt0a, tm], writes=[xd])
                    E(POOL, lambda e: e.tensor_tensor(out=xdw.ap[:, :].rearrange("p (h d) -> p h d", h=8), in0=xd.ap[:, :].rearrange("p (h d) -> p h d", h=8),
                                                      in1=wend.ap[:, 8 * g:8 * g + 8].unsqueeze(2).to_broadcast([128, 8, 64]), op=ALU.mult),
                      reads=[xd, wend], writes=[xdw])
                    E(ACT, lambda e: e.activation(out=bt.ap[:, :], in_=pt0b.ap[:, 0:128], func=AF.Copy), reads=[pt0b], writes=[bt])

                def dS(g):
                    xdw, bt = xdtw[g % 2], Btm[g % 2]
                    E(PE, lambda e: e.matmul(pt1.ap[:, :], lhsT=bt.ap[:, :], rhs=xdw.ap[:, :], start=True, stop=True), reads=[bt, xdw], writes=[pt1])

                def A(g, q4, bi):
                    h0 = 8 * g + 4 * q4
                    bqb = pm[bi % 2]
                    a1 = nxt("ft", fT)
                    a2, ee = t2b[bi % 2], e2b[bi % 2]
                    mh, cp = Mhb[bi % 3], Cpb[bi % 3]
                    for i4 in range(4):
                        E(PE, lambda e, h=h0 + i4, i4=i4: e.matmul(bqb.ap[:, i4 * 128:(i4 + 1) * 128], lhsT=selcol.ap[:, h:h + 1].to_broadcast([64, 128]),
                                                                   rhs=dtacs.ap[0:64, c0:c1], start=True, stop=True),
                          reads=[selcol, dtacs], writes=[bqb])
                    E(DVE, lambda e: e.tensor_tensor(out=a1.ap[:, :].rearrange("p (h l) -> p h l", h=4), in0=bqb.ap[:, :].rearrange("p (h l) -> p h l", h=4),
                                                     in1=tm.ap[:, 32 + h0:36 + h0].unsqueeze(2).to_broadcast([128, 4, 128]), op=ALU.subtract),
                      reads=[bqb, tm], writes=[a1])
                    E(DVE, lambda e: e.tensor_scalar(out=a1.ap[:, :], in0=a1.ap[:, :], scalar1=0.0, scalar2=None, op0=ALU.min), reads=[a1], writes=[a1])
                    E(ACT, lambda e: e.activation(out=a2.ap[:, :], in_=a1.ap[:, :], func=AF.Exp), reads=[a1], writes=[a2])
                    E(ACT, lambda e: e.activation(out=ee.ap[:, :], in_=bqb.ap[:, :], func=AF.Exp), reads=[bqb], writes=[ee])
                    E(DVE, lambda e: e.scalar_tensor_tensor(out=mh.ap[:, :].rearrange("p (h l) -> p h l", h=4), in0=a2.ap[:, :].rearrange("p (h l) -> p h l", h=4), scalar=1.0,
                                                            in1=cbm.ap[:, g, :].unsqueeze(1).to_broadcast([128, 4, 128]), op0=ALU.min, op1=ALU.mult),
                      reads=[a2, cbm], writes=[mh])
                    E(POOL, lambda e: e.tensor_tensor(out=cp.ap[:, :].rearrange("p (h l) -> p h l", h=4), in0=ee.ap[:, :].rearrange("p (h l) -> p h l", h=4),
                                                      in1=xbc[20 + g].ap[:, c0:c1].unsqueeze(1).to_broadcast([128, 4, 128]), op=ALU.mult),
                      reads=[ee, xbc[20 + g]], writes=[cp])

                def B(g, q4, bi):
                    ypm = pm[2 + g % 2]
                    xd = xdt[g % 2]
                    mh, cp = Mhb[bi % 3], Cpb[bi % 3]
                    for i4 in range(4):
                        hl = 4 * q4 + i4
                        h = 8 * g + hl
                        po = ypm.ap[(hl % 2) * 64:(hl % 2) * 64 + 64, (hl // 2) * 128:(hl // 2 + 1) * 128]
                        E(PE, lambda e, po=po, hl=hl, i4=i4: e.matmul(po, lhsT=xd.ap[:, hl * 64:(hl + 1) * 64], rhs=mh.ap[:, i4 * 128:(i4 + 1) * 128], start=True, stop=False),
                          reads=[xd, mh], writes=[ypm])
                        E(PE, lambda e, po=po, h=h, i4=i4: e.matmul(po, lhsT=stbf.ap[:, h * 64:(h + 1) * 64], rhs=cp.ap[:, i4 * 128:(i4 + 1) * 128], start=False, stop=True),
                          reads=[stbf_g[g], cp], writes=[ypm])

                def G_epi(g):
                    if doy:
                        ypm = pm[2 + g % 2]
                        for k in range(4):
                            ct = 4 * g + k
                            E(DVE, lambda e, ct=ct, k=k: e.scalar_tensor_tensor(out=yT[ct].ap[:, c0:c1], in0=xbc[ct].ap[:, c0:c1], scalar=dsk.ap[:, ct:ct + 1],
                                                                                in1=ypm.ap[:, k * 128:(k + 1) * 128], op0=ALU.mult, op1=ALU.add),
                              reads=[xbc[ct], dsk, ypm], writes=[yT[ct]], same_ok=True)
                    stg_b = state_h[8 * g:8 * g + 8]
                    E(DVE, lambda e: e.tensor_tensor(out=state.ap[:, g * 512:(g + 1) * 512].rearrange("p (h d) -> p h d", h=8), in0=state.ap[:, g * 512:(g + 1) * 512].rearrange("p (h d) -> p h d", h=8),
                                                     in1=elast.ap[:, 8 * g:8 * g + 8].unsqueeze(2).to_broadcast([128, 8, 64]), op=ALU.mult),
                      reads=stg_b + [elast], writes=stg_b)
                    E(DVE, lambda e: e.tensor_tensor(out=state.ap[:, g * 512:(g + 1) * 512], in0=state.ap[:, g * 512:(g + 1) * 512], in1=pt1.ap[:, :], op=ALU.add),
                      reads=stg_b + [pt1], writes=stg_b)
                    E(ACT, lambda e: e.activation(out=stbf.ap[:, g * 512:(g + 1) * 512], in_=state.ap[:, g * 512:(g + 1) * 512], func=AF.Copy), reads=stg_b, writes=[stbf_g[g]])

                C_pro()
                G_pro1(0)
                if not doy:
                    for g in range(4):
                        if g + 1 < 4:
                            G_pro1(g + 1)
                        dS(g)
                        G_epi(g)
                    return
                batches = [(g, q4) for g in range(4) for q4 in range(2)]
                dS(0)
                A(0, 0, 0)
                A(0, 1, 1)
                for bi, (g, q4) in enumerate(batches):
                    if bi + 2 < len(batches):
                        g2, q42 = batches[bi + 2]
                        if q42 == 0:
                            G_pro1(g2)
                        A(g2, q42, bi + 2)
                    B(g, q4, bi)
                    if q4 == 1:
                        G_epi(g)
                        if g + 1 < 4:
                            dS(g + 1)

            for ci in range(4):
                ssd_chunk(ci)
            if NP > 0 and tt == NP - 1:
                E(DVE, lambda e: e.tensor_scalar(out=state.ap[:, :], in0=state.ap[:, :], scalar1=pv.ap[:, 0:1], scalar2=None, op0=ALU.mult), reads=state_h + [pv], writes=state_h)
                E(ACT, lambda e: e.activation(out=stbf.ap[:, :], in_=state.ap[:, :], func=AF.Copy), reads=state_h, writes=stbf_g)
            if not mixfull:
                continue
            for ct in (0, 5, 15):
                dump("yraw%d" % ct, yT[ct], [128, TT], BF16, tt)

            def q_consume(j, pmb):
                E(ACT, lambda e: e.activation(out=qT[j].ap, in_=pmb.ap[:, :], func=AF.Copy), reads=[pmb], writes=[qT[j]])

            proj_fm("in", wb_in, 8, OFF_Q, 8, hT_src, q_consume, ts=TS)

            a_i = tt - (NP - 1) if NP > 0 else tt + 1
            for s in range(4):
                o = 2 * a_i + s // 2
                E(POOL, lambda e, s=s, o=o: e.tensor_copy(out=vbt.ap[:, s, :], in_=vtab.ap[:, o, :]), reads=[vtab], writes=[vbt], same_ok=True)
                E(POOL, lambda e, s=s, o=o: e.tensor_scalar(out=v01t.ap[:, s, :], in0=vtab.ap[:, o, :], scalar1=-1.0, scalar2=None, op0=ALU.is_ge), reads=[vtab], writes=[v01t], same_ok=True)
                E(POOL, lambda e, s=s, o=o: e.tensor_copy(out=ownt.ap[:, s, :], in_=own01.ap[:, o, :]), reads=[own01], writes=[ownt], same_ok=True)
            nsub_total = (tt + 1) * 4
            nun = (nsub_total + NSU - 1) // NSU
            def keep_sub(h, ksi):
                if ksi >= tt * 4:
                    return True
                return SLOPES[h] * (tt * TT - (ksi * 128 + 127)) <= ALIBI_CUT

            head_subs = [[(u, ks) for u in range(nun) for ks in range(min(NSU, nsub_total - u * NSU)) if keep_sub(h, u * NSU + ks)] for h in range(16)]
            units = [(h, u) for h in range(16) for u in sorted(set(u for u, _ in head_subs[h]))]
            unit_idx = {hu: j for j, hu in enumerate(units)}
            unit_buf = {}

            def load_unit(j):
                if j >= len(units) or j in unit_buf:
                    return
                h, u = units[j]
                bi_ = rr["ku"] % NKB
                kt, vt = Kt[bi_], Vt[bi_]
                rr["ku"] += 1
                ns = min(NSU, nsub_total - u * NSU)
                k0 = u * KU
                tl = sorted(set((k0 + i * 128) // TT for i in range(ns)))
                dma(SP, kt.ap[0:64, 0:ns * 128], Ks[h, :, k0:k0 + ns * 128], [KsB[t_] for t_ in tl], [Kt_k[bi_]])
                dma(SP, kt.ap[64:97, 0:ns * 128], kaug_d[:, k0:k0 + ns * 128], [DIN], [Kt_a[bi_]])
                dma(SP, vt.ap[:, 0:ns, 0:64], Vs[h, :, u * NSU:u * NSU + ns, :], [VsB[t_] for t_ in tl], [vt])
                unit_buf[j] = (kt, vt, [Kt_k[bi_], Kt_a[bi_]])

            def prep_head(h):
                hp, pb = h // 2, 64 * (h % 2)
                qa = Qaug[h % 2]
                dma(SP, qa.ap[0:64, :], qT[hp].ap[pb:pb + 64, :], [qT[hp]], [qa])
                for s in SR:
                    E(PE, lambda e, s=s, hp=hp, pb=pb: e.matmul(pa0c.ap[:, s * 32:(s + 1) * 32], lhsT=qT[hp].ap[pb:pb + 64, s * 128:(s + 1) * 128], rhs=kmT.ap[pb:pb + 64, hp, :],
                                                                start=True, stop=True),
                      reads=[qT[hp], kmT], writes=[pa0c])
                S0 = TS // 128
                E(DVE, lambda e: e.tensor_tensor(out=gm.ap[:, S0:4, :], in0=pa0c.ap[:, :].rearrange("p (s n) -> p s n", s=4)[:, S0:4, :], in1=vbt.ap[:, S0:4, :], op=ALU.add),
                  reads=[pa0c, vbt], writes=[gm])
                for s in SR:
                    E(DVE, lambda e, s=s: e.max(out=m8.ap[:, s, :], in_=gm.ap[:, s, :]), reads=[gm], writes=[m8], same_ok=(s > SR[0]))
                E(DVE, lambda e: e.tensor_tensor(out=selb.ap[:, S0:4, :], in0=gm.ap[:, S0:4, :], in1=m8.ap[:, S0:4, 2:3].to_broadcast([128, 4 - S0, 32]), op=ALU.is_ge),
                  reads=[gm, m8], writes=[selb])
                E(DVE, lambda e: e.tensor_tensor(out=selb.ap[:, S0:4, :], in0=selb.ap[:, S0:4, :], in1=v01t.ap[:, S0:4, :], op=ALU.mult), reads=[selb, v01t], writes=[selb])
                E(DVE, lambda e: e.tensor_tensor(out=selb.ap[:, S0:4, :], in0=selb.ap[:, S0:4, :], in1=ownt.ap[:, S0:4, :], op=ALU.add), reads=[selb, ownt], writes=[selb])
                E(DVE, lambda e: e.tensor_scalar(out=gstage.ap[:, S0:4, 64:96], in0=selb.ap[:, S0:4, :], scalar1=BIG, scalar2=-BIG, op0=ALU.mult, op1=ALU.add),
                  reads=[selb], writes=[gstage])
                E(DVE, lambda e, h=h: e.tensor_copy(out=gstage.ap[:, :, 96:97], in_=qsh.ap[:, h, :].unsqueeze(2)), reads=[qsh], writes=[gstage])
                if h == 3:
                    dump("selb", selb, [128, 4, 32], F32, tt)
                    dump("gm", gm, [128, 4, 32], F32, tt)
                for s in SR:
                    E(PE, lambda e, s=s: e.transpose(out=pt0a.ap[0:97, s * 128:(s + 1) * 128], in_=gstage.ap[:, s, :], identity=identb.ap[:]),
                      reads=[gstage, identb], writes=[pt0a])
                E(ACT, lambda e, TS=TS, qa=qa: e.activation(out=qa.ap[64:97, TS:TT], in_=pt0a.ap[64:97, TS:TT], func=AF.Copy), reads=[pt0a], writes=[qa])

            LOOK = 2
            spm = pm[0:3]
            load_unit(0)
            load_unit(1)
            load_unit(2)
            prep_head(0)
            def z_consume(j, pmb):
                tmp = nxt("ft", fT)
                si = rr["stg"] % 3
                rr["stg"] += 1
                sqb = stage[si]
                sq_bufs = [stage_h[si], stage_d[si]]

                def s0():
                    E(ACT, lambda e: e.activation(out=tmp.ap[:, :], in_=pmb.ap[:, :], func=AF.Silu), reads=[pmb], writes=[tmp])
                    E(DVE, lambda e: e.tensor_tensor(out=yT[j].ap, in0=yT[j].ap, in1=tmp.ap[:, :], op=ALU.mult), reads=[yT[j], tmp], writes=[yT[j]])
                    E(POOL, lambda e: e.tensor_tensor(out=sqb.ap[:, 0:TT], in0=yT[j].ap, in1=yT[j].ap, op=ALU.mult), reads=[yT[j]], writes=sq_bufs)

                def s1():
                    E(PE, lambda e: e.matmul(pa1.ap[:, :], lhsT=onesb.ap[:, :], rhs=sqb.ap[:, 0:TT], start=(j % 4 == 0), stop=(j % 4 == 3)), reads=[onesb] + sq_bufs, writes=[pa1])
                    if j % 4 == 3:
                        rs = nxt("ft", fT)
                        E(ACT, lambda e: e.activation(out=rs.ap[:, :], in_=pa1.ap[:, :], func=AF.Ln, scale=1.0 / 512, bias=EPS), reads=[pa1], writes=[rs])
                        E(ACT, lambda e: e.activation(out=rs.ap[:, :], in_=rs.ap[:, :], func=AF.Exp, scale=-0.5), reads=[rs], writes=[rs])
                        for k in range(4):
                            ct = j - 3 + k
                            E(DVE, lambda e, ct=ct: e.scalar_tensor_tensor(out=yT[ct].ap, in0=yT[ct].ap, scalar=gon.ap[:, ct:ct + 1], in1=rs.ap[:, :], op0=ALU.mult, op1=ALU.mult),
                              reads=[yT[ct], gon, rs], writes=[yT[ct]])

                return [s0, s1]

            proj_fm("in", wb_in, 8, OFF_Z, 16, hT_src, z_consume, ts=TS)
            for ct in (0, 5, 15):
                dump("yn%d" % ct, yT[ct], [128, TT], BF16, tt)

            for h in range(16):
                qa = Qaug[h % 2]
                acc = pa1 if h % 2 == 0 else pm[3]
                subs = head_subs[h]
                n = len(subs)
                spbs = {}

                def emit_qk(i, h=h, qa=qa, subs=subs, spbs=spbs):
                    u, ks = subs[i]
                    j = unit_idx[(h, u)]
                    if j not in unit_buf or i == 0 or subs[i - 1][0] != u:
                        load_unit(j)
                        load_unit(j + 1)
                    kt, vt, ktb = unit_buf[j]
                    ksi = u * NSU + ks
                    in_tile = ksi >= tt * 4
                    spb = spm[rr["pm"] % 3]
                    rr["pm"] += 1
                    E(PE, lambda e, TS=TS, ks=ks, kt=kt, qa=qa, spb=spb, in_tile=in_tile: e.matmul(spb.ap[:, TS:TT], lhsT=kt.ap[0:97, ks * 128:(ks + 1) * 128], rhs=qa.ap[0:97, TS:TT],
                                                                                             start=True, stop=(not in_tile)),
                      reads=ktb + [qa], writes=[spb])
                    if in_tile:
                        cidx = ksi - tt * 4
                        E(PE, lambda e, TS=TS, cidx=cidx, spb=spb: e.matmul(spb.ap[:, TS:TT], lhsT=identb.ap[:, :], rhs=cmask.ap[:, cidx, TS:TT], start=False, stop=True),
                          reads=[identb, cmask], writes=[spb])
                    spbs[i] = spb

                for i in range(min(LOOK, n)):
                    emit_qk(i)
                for i in range(n):
                    if i + LOOK < n:
                        emit_qk(i + LOOK)
                    if i == min(4, n - 1) and h + 1 < 16:
                        prep_head(h + 1)
                    u, ks = subs[i]
                    kt, vt, ktb = unit_buf[unit_idx[(h, u)]]
                    ksi = u * NSU + ks
                    spb = spbs[i]
                    ptb = pT[rr["pt"] % len(pT)]
                    rr["pt"] += 1
                    off = ksi - tt * 4 + NOFFMAX
                    E(ACT, lambda e, TS=TS, spb=spb, ptb=ptb, h=h, off=off: e.activation(out=ptb.ap[:, TS:TT], in_=spb.ap[:, TS:TT], func=AF.Exp, scale=0.125, bias=abias.ap[:, h, off:off + 1]),
                      reads=[spb, abias], writes=[ptb])
                    E(PE, lambda e, TS=TS, ks=ks, vt=vt, ptb=ptb, acc=acc, st_=(i == 0), sp_=(i == n - 1): e.matmul(acc.ap[0:65, TS:TT], lhsT=vt.ap[:, ks, 0:65], rhs=ptb.ap[:, TS:TT], start=st_, stop=sp_),
                      reads=[vt, ptb], writes=[acc])
                numer, den = nxt("ft", fT), nxt("ft", fT)
                E(ACT, lambda e, TS=TS, numer=numer, acc=acc: e.activation(out=numer.ap[0:64, TS:TT], in_=acc.ap[0:64, TS:TT], func=AF.Copy), reads=[acc], writes=[numer])
                E(ACT, lambda e, TS=TS, den=den, acc=acc: e.activation(out=den.ap[64:65, TS:TT], in_=acc.ap[64:65, TS:TT], func=AF.Copy), reads=[acc], writes=[den])
                E(PE, lambda e, TS=TS, den=den: e.matmul(pt1.ap[0:64, TS:TT], lhsT=onesf.ap[64:65, 0:64], rhs=den.ap[64:65, TS:TT], start=True, stop=True), reads=[onesf, den], writes=[pt1])
                E(DVE, lambda e, TS=TS, den=den: e.reciprocal(out=den.ap[0:64, TS:TT], in_=pt1.ap[0:64, TS:TT]), reads=[pt1, den], writes=[den])
                E(DVE, lambda e, TS=TS, h=h, numer=numer, den=den: e.tensor_tensor(out=attT[h].ap[:, TS:TT], in0=numer.ap[0:64, TS:TT], in1=den.ap[0:64, TS:TT], op=ALU.mult), reads=[numer, den], writes=[attT[h]])
            for h in (0, 3, 15):
                dump("att%d" % h, attT[h], [64, TT], BF16, tt)

            sgs, sga, tmx = G[0:8], G[8:16], G[16:24]

            def gs_consume(j, pmb):
                E(ACT, lambda e: e.activation(out=sgs[j].ap, in_=pmb.ap[:, :], func=AF.Sigmoid), reads=[pmb], writes=[sgs[j]])

            def ga_consume(j, pmb):
                E(ACT, lambda e: e.activation(out=sga[j].ap, in_=pmb.ap[:, :], func=AF.Sigmoid), reads=[pmb], writes=[sga[j]])

            proj_fm("in", wb_in, 8, OFF_GS, 8, hT_src, gs_consume, ts=TS)
            proj_fm("in", wb_in, 8, OFF_GA, 8, hT_src, ga_consume, ts=TS)
            yT_src = Src([yT[c].ap for c in range(16)], yT)

            def ys_consume(j, pmb):
                E(DVE, lambda e: e.tensor_tensor(out=tmx[j].ap, in0=pmb.ap[:, :], in1=sgs[j].ap, op=ALU.mult), reads=[pmb, sgs[j]], writes=[tmx[j]])

            proj_fm("ssd", wb_ssd, 16, 0, 8, yT_src, ys_consume, ts=TS)
            mixT = yT[0:8]
            for p0 in (0, 2, 4, 6):
                wbf = nxt("wb", wbufs)
                view = wbf.ap[0:64, :].rearrange("p (c n) -> p c n", c=16)
                dma(SP, view[:, :, 0:256], wb_attn[:, p0 * 128:p0 * 128 + 256].rearrange("(h d) n -> d h n", d=64), wdeps("attn", 0, D, p0 * 128, p0 * 128 + 256), [wbf])
                for j in range(2):
                    jj = p0 + j
                    pmb = nxt("pm", pm)
                    for hh in range(16):
                        E(PE, lambda e, TS=TS, hh=hh, j=j, pmb=pmb, view=view: e.matmul(pmb.ap[:, TS:TT], lhsT=view[:, hh, j * 128:(j + 1) * 128], rhs=attT[hh].ap[:, TS:TT], start=(hh == 0), stop=(hh == 15)),
                          reads=[wbf, attT[hh]], writes=[pmb])
                    tmp = nxt("ft", fT)
                    E(DVE, lambda e, jj=jj, pmb=pmb, tmp=tmp: e.tensor_tensor(out=tmp.ap[:, :], in0=pmb.ap[:, :], in1=sga[jj].ap, op=ALU.mult), reads=[pmb, sga[jj]], writes=[tmp])
                    E(POOL, lambda e, jj=jj, tmp=tmp: e.tensor_tensor(out=mixT[jj].ap, in0=tmp.ap[:, :], in1=tmx[jj].ap, op=ALU.add), reads=[tmp, tmx[jj]], writes=[mixT[jj]])
            for ct in (0, 7):
                dump("mix%d" % ct, mixT[ct], [128, TT], BF16, tt)

            def post_norm_residual(rb, gain_bc, s, dst_ap, dst_bufs):
                E(ACT, lambda e: e.activation(out=junk.ap[:, :], in_=rb.ap[:, :], func=AF.Square, accum_out=ssq.ap[:, 0:1]), reads=[rb], writes=[junk, ssq])
                E(ACT, lambda e: e.activation(out=rstd.ap[:, 0:1], in_=ssq.ap[:, 0:1], func=AF.Ln, scale=1.0 / D, bias=EPS), reads=[ssq], writes=[rstd])
                E(ACT, lambda e: e.activation(out=rstd.ap[:, 0:1], in_=rstd.ap[:, 0:1], func=AF.Exp, scale=-0.5), reads=[rstd], writes=[rstd])
                E(DVE, lambda e: e.scalar_tensor_tensor(out=rb.ap[:, :], in0=rb.ap[:, :], scalar=rstd.ap[:, 0:1], in1=gain_bc.ap[:, :], op0=ALU.mult, op1=ALU.mult),
                  reads=[rb, rstd, gain_bc], writes=[rb])
                E(POOL, lambda e: e.tensor_tensor(out=dst_ap, in0=xt.ap[:, s, :], in1=rb.ap[:, :], op=ALU.add), reads=[xt_s[s], rb], writes=dst_bufs)

            wo = [load_panel("out", wb_out, 8, hf * 512, 512) for hf in range(2)]
            for s in SR:
                rb = rbufs[s % 2]
                for hf in range(2):
                    wbf, view = wo[hf]
                    pmb = nxt("pm", pm)
                    for c in range(8):
                        E(PE, lambda e, c=c, s=s, pmb=pmb, view=view: e.matmul(pmb.ap[:, :], lhsT=mixT[c].ap[:, s * 128:(s + 1) * 128], rhs=view[:, c, 0:512], start=(c == 0), stop=(c == 7)),
                          reads=[wbf, mixT[c]], writes=[pmb])
                    if hf == 0:
                        E(DVE, lambda e, pmb=pmb, rb=rb: e.tensor_copy(out=rb.ap[:, 0:512], in_=pmb.ap[:, :]), reads=[pmb], writes=[rb])
                    else:
                        E(ACT, lambda e, pmb=pmb, rb=rb: e.activation(out=rb.ap[:, 512:1024], in_=pmb.ap[:, :], func=AF.Copy), reads=[pmb], writes=[rb])
                post_norm_residual(rb, gqm, s, xt.ap[:, s, :], [xt_s[s]])
            dump("x1", xt, [128, 4, D], F32, tt, bufs=xt_s)

            norm_transpose(gpf)
            semi = not is_main

            ftx = [Buf(rbufs[i // 2].ap[:, (i % 2) * TT:(i % 2 + 1) * TT]) for i in range(4)]
            for a_ in ftx:
                a_.r = [o_ for p_ in rbufs for o_ in (p_.r + ([p_.w] if p_.w is not None else []))]
            fT8 = fT + ftx

            def gate_consume(j, pmb):
                si = rr["stg"] % 3
                rr["stg"] += 1
                stg, sh, sd = stage[si], stage_h[si], stage_d[si]

                def s0():
                    E(POOL, lambda e: e.tensor_copy(out=stg.ap[:, 1:3], in_=fhalo[j].ap), reads=[fhalo[j]], writes=[sh])
                    E(ACT, lambda e: e.activation(out=stg.ap[:, 3:TT + 3], in_=pmb.ap[:, :], func=AF.Copy), reads=[pmb], writes=[sd])
                    E(POOL, lambda e: e.tensor_copy(out=fhalo[j].ap, in_=stg.ap[:, TT + 1:TT + 3]), reads=[sd], writes=[fhalo[j]])

                if semi:
                    return [s0]
                tmp, x2 = fT8[rr["f8"] % 8], fT8[(rr["f8"] + 1) % 8]
                rr["f8"] += 2

                def s0b():
                    s0()
                    E(ACT, lambda e: e.activation(out=tmp.ap[:, :], in_=stg.ap[:, 1:TT + 1], func=AF.Identity, scale=fcw.ap[:, j, 0:1], bias=fcb.ap[:, j:j + 1]),
                      reads=[sh, sd, fcw, fcb], writes=[tmp])

                def s1():
                    for k in range(1, 3):
                        E(DVE, lambda e, k=k: e.scalar_tensor_tensor(out=tmp.ap[:, :], in0=stg.ap[:, 1 + k:1 + k + TT], scalar=fcw.ap[:, j, k:k + 1], in1=tmp.ap[:, :], op0=ALU.mult, op1=ALU.add),
                          reads=[sh, sd, fcw, tmp], writes=[tmp])
                    E(ACT, lambda e: e.activation(out=x2.ap[:, :], in_=tmp.ap[:, :], func=AF.Square, scale=0.21145921592590755), reads=[tmp], writes=[x2])

                def s2():
                    E(DVE, lambda e: e.scalar_tensor_tensor(out=x2.ap[:, :], in0=x2.ap[:, :], scalar=1.0, in1=tmp.ap[:, :], op0=ALU.add, op1=ALU.mult), reads=[x2, tmp], writes=[x2])
                    E(ACT, lambda e: e.activation(out=x2.ap[:, :], in_=x2.ap[:, :], func=AF.Sigmoid, scale=1.5957691216057308), reads=[x2], writes=[x2])

                def s3():
                    E(DVE, lambda e: e.tensor_tensor(out=act[j].ap, in0=tmp.ap[:, :], in1=x2.ap[:, :], op=ALU.mult), reads=[tmp, x2], writes=[act[j]])

                return [s0b, s1, s2, s3]

            proj_fm("up", wb_up, 8, 0, 32, hT_src, gate_consume, ts=TS)
            for p_ in rbufs:
                for a_ in ftx:
                    p_.r.extend(a_.r)
                    if a_.w is not None:
                        p_.r.append(a_.w)
            if semi:
                E(DVE, lambda e: e.tensor_scalar(out=fhalo_all.ap[:, :, :], in0=fhalo_all.ap[:, :, :], scalar1=pv.ap[:, 0:1], scalar2=None, op0=ALU.mult), reads=fhalo + [pv], writes=fhalo)
                continue

            def up_consume(j, pmb):
                E(DVE, lambda e: e.tensor_tensor(out=act[j].ap, in0=pmb.ap[:, :], in1=act[j].ap, op=ALU.mult), reads=[pmb, act[j]], writes=[act[j]])

            proj_fm("up", wb_up, 8, 4096, 32, hT_src, up_consume)
            for ct in (0, 31):
                dump("act%d" % ct, act[ct], [128, TT], BF16, tt)
            for sp2 in range(2):
                accs = {(s, hf): pm[(s % 2) * 2 + hf] for s in (2 * sp2, 2 * sp2 + 1) for hf in range(2)}
                for kp in range(8):
                    wbf = nxt("wb", wbufs)
                    view = wbf.ap[:, :].rearrange("p (c n) -> p c n", c=4)
                    dma(SP, view, wb_down[kp * 512:(kp + 1) * 512, :].rearrange("(c p) n -> p c n", p=128), wdeps("down", kp * 512, (kp + 1) * 512, 0, D), [wbf])
                    for (s, hf), acc in accs.items():
                        for c in range(4):
                            kc = kp * 4 + c
                            E(PE, lambda e, s=s, hf=hf, acc=acc, c=c, kc=kc, view=view: e.matmul(acc.ap[:, :], lhsT=act[kc].ap[:, s * 128:(s + 1) * 128], rhs=view[:, c, hf * 512:(hf + 1) * 512],
                                                                                                  start=(kc == 0), stop=(kc == 31)),
                              reads=[wbf, act[kc]], writes=[acc])
                for s in (2 * sp2, 2 * sp2 + 1):
                    rb = rbufs[s % 2]
                    E(DVE, lambda e, a0=accs[(s, 0)], rb=rb: e.tensor_copy(out=rb.ap[:, 0:512], in_=a0.ap[:, :]), reads=[accs[(s, 0)]], writes=[rb])
                    E(ACT, lambda e, a1_=accs[(s, 1)], rb=rb: e.activation(out=rb.ap[:, 512:1024], in_=a1_.ap[:, :], func=AF.Copy), reads=[accs[(s, 1)]], writes=[rb])
                    post_norm_residual(rb, gqf, s, rb.ap[:, :], [rb])
                    r0 = tm_i * TT + s * 128
                    final_ops.append(dma(SP, out_d[r0:r0 + 128, :], rb.ap[:, :], [rb], [Buf(None)]))
        counts = P.finalize(final_ops + dbg_final)
    return nc, counts, dbg_out


def _slopes():
    return (2.0 ** (-8.0 * np.arange(1, 17, dtype=np.float64) / 16)).astype(np.float64)


def _consts(NP, NM, second_half):
    NPB, NLB = 2 * NP, 2 * NM
    W = (NP + NM) * TT
    NOFFMAX = (NP + NM - 1) * 4
    NOFF = NOFFMAX + 4
    NUNITS = (W + KU - 1) // KU
    c = {}
    c["identb"] = np.eye(128, dtype=np.float32).astype(NPBF)
    c["identf"] = np.eye(64, dtype=np.float32)
    sel = np.zeros((64, 32), np.float32)
    sel[32 + np.arange(32), np.arange(32)] = 1.0
    c["selcol"] = sel
    c["triu"] = np.triu(np.ones((128, 128), np.float32))
    p = np.arange(128)[:, None, None]
    ci = np.arange(4)[None, :, None]
    q = np.arange(512)[None, None, :]
    c["cmask"] = np.where(ci * 128 + p > q, -BIG, 0.0).astype(np.float32).astype(NPBF)
    keys = np.arange(NUNITS * KU)
    ka = np.zeros((33, NUNITS * KU), np.float32)
    blk = keys // 256
    for r in range(32):
        ka[r, blk == r] = 1.0
    ka[32, :] = 1.0
    c["kaug"] = ka.astype(NPBF)
    sl = _slopes()
    pp = np.arange(128)[:, None, None]
    c["qsh"] = (-8.0 * sl[None, :, None] * (np.arange(4)[None, None, :] * 128 + pp)).astype(np.float32)
    c["abias"] = (sl[None, :, None] * (pp + 128.0 * (np.arange(NOFF)[None, None, :] - NOFFMAX))).astype(np.float32)
    NO = NLB + 2
    vt = np.full((NO, 32), -1e30, np.float32)
    ow = np.zeros((NO, 32), np.float32)
    pvb = 0.0 if second_half else -1e30
    for o in range(NO):
        n_own = NPB - 2 + o
        for n in range(32):
            if n < n_own:
                vt[o, n] = pvb if n < NPB else 0.0
        if 0 <= n_own < 32:
            ow[o, n_own] = 1.0
    c["vtab"] = np.ascontiguousarray(np.broadcast_to(vt[None], (128, NO, 32)))
    c["own01"] = np.ascontiguousarray(np.broadcast_to(ow[None], (128, NO, 32)))
    c["pv"] = np.full((128, 1), 1.0 if second_half else 0.0, np.float32)
    return c


def _params(inp):
    f = lambda a: np.ascontiguousarray(np.asarray(a, dtype=np.float32))
    pr = {}
    pr["w_in"] = f(inp["w_in"][0])
    pr["w_ssd"] = f(inp["w_ssd_branch"][0])
    pr["w_attn"] = f(inp["w_attn_branch"][0])
    pr["w_out"] = f(inp["w_out"][0])
    pr["w_up"] = f(inp["w_ffn_up"][0])
    pr["w_down"] = f(inp["w_ffn_down"][0])
    pr["g_pre_mix"] = f(np.asarray(inp["pre_mix_norm"][0]).reshape(8, 128).T)
    pr["g_pre_ffn"] = f(np.asarray(inp["pre_ffn_norm"][0]).reshape(8, 128).T)
    pr["g_post_mix"] = f(np.asarray(inp["post_mix_norm"][0]).reshape(1, D))
    pr["g_post_ffn"] = f(np.asarray(inp["post_ffn_norm"][0]).reshape(1, D))
    pr["cw"] = f(np.asarray(inp["ssd_conv_w"][0]).T.reshape(24, 128, 4).transpose(1, 0, 2))
    pr["cb"] = f(np.asarray(inp["ssd_conv_b"][0]).reshape(24, 128).T)
    pr["dtb"] = f(np.tile(np.asarray(inp["ssd_dt_bias"][0]), 2).reshape(64, 1))
    pr["alog"] = f(np.tile(np.asarray(inp["ssd_a_log"][0]), 2).reshape(64, 1))
    pr["dsk"] = f(np.repeat(np.asarray(inp["ssd_d_skip"][0]), 64).reshape(16, 128).T)
    pr["gon"] = f(np.asarray(inp["ssd_out_norm"][0]).reshape(16, 128).T)
    pr["fcw"] = f(np.asarray(inp["ffn_conv_w"][0]).T.reshape(32, 128, 3).transpose(1, 0, 2))
    pr["fcb"] = f(np.asarray(inp["ffn_conv_b"][0]).reshape(32, 128).T)
    return pr


def run_cores(inp, NP, NM, cores, dbg=(), dbg_tile=None):
    nc, counts, dbg_out = build_nc(NP, NM, dbg=dbg, dbg_tile=dbg_tile)
    pr = _params(inp)
    x = np.asarray(inp["x"], dtype=np.float32)
    W = (NP + NM) * TT
    in_maps = []
    for (b, hf) in cores:
        m = dict(pr)
        m.update(_consts(NP, NM, hf == 1))
        xw = np.zeros((W, D), np.float32)
        if hf == 0:
            xw[NP * TT:] = x[b, :NM * TT]
        else:
            xw[:] = x[b, :W]
        m["xw"] = xw
        in_maps.append(m)
    res = run_bass_kernel_spmd(nc, in_maps, core_ids=list(range(len(cores))))
    return res, counts


def kernel(**inputs):
    NP, NM = 8, 8
    cores = [(b, hf) for b in range(4) for hf in range(2)]
    res, _ = run_cores(inputs, NP, NM, cores)
    x = np.asarray(inputs["x"])
    out = np.empty(x.shape, np.float32)
    for i, (b, hf) in enumerate(cores):
        out[b, hf * NM * TT:(hf + 1) * NM * TT] = res.results[i]["out"]
    return out
```
